# Optimizing a Trainium2 kernel written in Bass

```python
import jax, jax.numpy as jnp
from jax import lax
import numpy as np

D_MODEL = 1024
BATCH = 8
SEQ = 4096
DEPTH = 1

CTX_LEN = 256
GRID_W = 64
EPS = 1e-6
CHUNK = 128
A_GROUPS = 8
A_WIDTH = 1024
A_GROUP_DIM = A_WIDTH // A_GROUPS
N_HEADS = 16
N_KV_HEADS = 4
HEAD_DIM = 64
B_WIDTH = N_HEADS * HEAD_DIM
KV_WIDTH = N_KV_HEADS * HEAD_DIM
WINDOW = 128
BLOCK = 128
ROPE_BASE = 10000.0
N_EXPERTS = 16
CAPACITY_FACTOR = 2
D_EXPERT = 2048
OFF_U = 0
OFF_VA = OFF_U + A_WIDTH
OFF_Q = OFF_VA + A_WIDTH
OFF_K = OFF_Q + B_WIDTH
OFF_VB = OFF_K + KV_WIDTH
OFF_GATE = OFF_VB + KV_WIDTH
IN_COLS = OFF_GATE + 2 * D_MODEL

kernel_name = "hybrid_gmlp_window_gqa_ec_moe_dit"


def rmsnorm(x, g):
    xf = x.astype(jnp.float32)
    y = xf * lax.rsqrt(jnp.mean(xf * xf, axis=-1, keepdims=True) + EPS)
    return (y * g.astype(jnp.float32)).astype(x.dtype)


def layernorm(x, g, b):
    xf = x.astype(jnp.float32)
    mu = jnp.mean(xf, axis=-1, keepdims=True)
    var = jnp.mean(jnp.square(xf - mu), axis=-1, keepdims=True)
    y = (xf - mu) * lax.rsqrt(var + EPS)
    return (y * g.astype(jnp.float32) + b.astype(jnp.float32)).astype(x.dtype)


def modulate(h, shift, scale):
    return h * (1 + scale) + shift


def _axis_angles(pos, dim):
    freqs = ROPE_BASE ** (-jnp.arange(0, dim, 2, dtype=jnp.float32) / dim)
    return pos.astype(jnp.float32)[:, None] * freqs[None, :]


def _rotate(x, ang):
    x1, x2 = jnp.split(x.astype(jnp.float32), 2, axis=-1)
    cos = jnp.cos(ang)[None, :, None, :]
    sin = jnp.sin(ang)[None, :, None, :]
    return jnp.concatenate([x1 * cos - x2 * sin, x1 * sin + x2 * cos], axis=-1)


def rope_2d(x, row, col):
    half = x.shape[-1] // 2
    xr, xc = x[..., :half], x[..., half:]
    out = jnp.concatenate([_rotate(xr, _axis_angles(row, half)),
                           _rotate(xc, _axis_angles(col, half))], axis=-1)
    return out.astype(x.dtype)


def chunk_gmlp(u, v, ln_g, ln_b, w_s, b_s):
    Bn, L, _ = v.shape
    v = layernorm(v, ln_g, ln_b)
    vc = v.reshape(Bn, L // CHUNK, CHUNK, A_GROUPS, A_GROUP_DIM)
    mixed = jnp.einsum('gpq,bnqgd->bnpgd', w_s, vc) + b_s.T[None, None, :, :, None]
    return u * mixed.reshape(Bn, L, A_WIDTH)


def window_gqa(q, k, v, k_ctx, v_ctx, sink):
    Bn, L, H, Dh = q.shape
    nb = L // BLOCK
    rep = H // N_KV_HEADS
    scale = HEAD_DIM ** -0.5
    qb = q.reshape(Bn, nb, BLOCK, N_KV_HEADS, rep, Dh)

    def band(t):
        tb = t.reshape(Bn, nb, BLOCK, N_KV_HEADS, Dh)
        tp = jnp.pad(tb, ((0, 0), (1, 1), (0, 0), (0, 0), (0, 0)))
        return jnp.concatenate([tp[:, :-2], tp[:, 1:-1], tp[:, 2:]], axis=2)

    kb, vb = band(k), band(v)
    a_idx = jnp.arange(BLOCK)[:, None]
    s_idx = jnp.arange(3 * BLOCK)[None, :]
    rel = s_idx - BLOCK - a_idx
    sink_l = sink.astype(jnp.float32).reshape(N_KV_HEADS, rep)[None, :, :, None, None]
    vals_ctx = v_ctx.astype(jnp.float32)

    def one_block(args):
        n, qn, kn, vn = args
        key_pos = (n - 1) * BLOCK + s_idx
        mask = (jnp.abs(rel) <= WINDOW) & (key_pos >= 0) & (key_pos < L)
        s_loc = jnp.einsum('bqkrd,bskd->bkrqs', qn, kn, preferred_element_type=jnp.float32) * scale
        s_loc = jnp.where(mask, s_loc, jnp.float32(-1e30))
        s_ctx = jnp.einsum('bqkrd,bskd->bkrqs', qn, k_ctx, preferred_element_type=jnp.float32) * scale
        logits = jnp.concatenate([s_loc, s_ctx], axis=-1)
        m = jnp.maximum(jnp.max(logits, axis=-1, keepdims=True), sink_l)
        p = jnp.exp(logits - m)
        denom = jnp.sum(p, axis=-1, keepdims=True) + jnp.exp(sink_l - m)
        vals = jnp.concatenate([vn.astype(jnp.float32), vals_ctx], axis=1)
        out = jnp.einsum('bkrqs,bskd->bqkrd', p / denom, vals)
        return out.astype(q.dtype)

    xs = (jnp.arange(nb), jnp.moveaxis(qb, 1, 0), jnp.moveaxis(kb, 1, 0), jnp.moveaxis(vb, 1, 0))
    out = lax.map(one_block, xs)
    return jnp.moveaxis(out, 0, 1).reshape(Bn, L, H * Dh)


def expert_choice_ffn(h, w_router, w_gate, w_up, w_down):
    Bn, N, D = h.shape
    cap = max(1, CAPACITY_FACTOR * N // N_EXPERTS)
    logits = jnp.einsum('bnd,de->bne', h, w_router, preferred_element_type=jnp.float32)
    aff = jax.nn.softmax(logits, axis=-1)
    top_w, top_idx = lax.top_k(jnp.swapaxes(aff, 1, 2), cap)
    xe = jax.vmap(lambda hb, ib: hb[ib])(h, top_idx)
    g = jnp.einsum('becd,edf->becf', xe, w_gate)
    u = jnp.einsum('becd,edf->becf', xe, w_up)
    y = jnp.einsum('becf,efd->becd', jax.nn.silu(g) * u, w_down)
    y = y * top_w[..., None].astype(y.dtype)
    return jax.vmap(lambda ib, yb: jnp.zeros((N, D), yb.dtype).at[ib.reshape(-1)].add(yb.reshape(-1, D)))(top_idx, y)


def setup_inputs(seed: int = 0) -> dict:
    key = jax.random.key(seed)
    ks = jax.random.split(key, 24)
    f32 = jnp.float32
    D = D_MODEL

    def nrm(k, shape, s):
        return jax.random.normal(k, shape, f32) * s

    return {
        "x": nrm(ks[0], (BATCH, SEQ, D), 1.0),
        "c": nrm(ks[1], (BATCH, D), 1.0),
        "ctx": nrm(ks[2], (BATCH, CTX_LEN, D), 1.0),
        "c_ctx": nrm(ks[3], (D,), 1.0),
        "w_ada": nrm(ks[4], (DEPTH, D, 6 * D), 0.3 * D ** -0.5),
        "b_ada": nrm(ks[5], (DEPTH, 6 * D), 0.02),
        "norm1_g": 1.0 + nrm(ks[6], (DEPTH, D), 0.05),
        "w_in": nrm(ks[7], (DEPTH, D, IN_COLS), D ** -0.5),
        "b_merge_gate": nrm(ks[8], (DEPTH, 2 * D), 0.02),
        "gmlp_ln_g": 1.0 + nrm(ks[9], (DEPTH, A_WIDTH), 0.05),
        "gmlp_ln_b": nrm(ks[10], (DEPTH, A_WIDTH), 0.02),
        "w_spatial": nrm(ks[11], (DEPTH, A_GROUPS, CHUNK, CHUNK), CHUNK ** -0.5),
        "b_spatial": nrm(ks[12], (DEPTH, A_GROUPS, CHUNK), 0.02),
        "attn_sink": nrm(ks[13], (DEPTH, N_HEADS), 0.5),
        "w_proj_a": nrm(ks[14], (DEPTH, A_WIDTH, D), A_WIDTH ** -0.5),
        "w_proj_b": nrm(ks[15], (DEPTH, B_WIDTH, D), B_WIDTH ** -0.5),
        "w_out": nrm(ks[16], (DEPTH, D, D), D ** -0.5),
        "norm2_g": 1.0 + nrm(ks[17], (DEPTH, D), 0.05),
        "w_router": nrm(ks[18], (DEPTH, D, N_EXPERTS), D ** -0.5),
        "w_exp_gate": nrm(ks[19], (DEPTH, N_EXPERTS, D, D_EXPERT), D ** -0.5),
        "w_exp_up": nrm(ks[20], (DEPTH, N_EXPERTS, D, D_EXPERT), D ** -0.5),
        "w_exp_down": nrm(ks[21], (DEPTH, N_EXPERTS, D_EXPERT, D), D_EXPERT ** -0.5),
        "final_g": 1.0 + nrm(ks[22], (D,), 0.05),
    }


def reference(x, c, ctx, c_ctx, w_ada, b_ada, norm1_g, w_in, b_merge_gate, gmlp_ln_g, gmlp_ln_b,
              w_spatial, b_spatial, attn_sink, w_proj_a, w_proj_b, w_out, norm2_g, w_router,
              w_exp_gate, w_exp_up, w_exp_down, final_g):
    Bn, L, D = x.shape
    rows = L // GRID_W
    row = jnp.repeat(jnp.arange(rows), GRID_W)
    col = jnp.tile(jnp.arange(GRID_W), rows)

    for l in range(DEPTH):
        mod = jax.nn.silu(c) @ w_ada[l] + b_ada[l]
        sh1, sc1, g1, sh2, sc2, g2 = jnp.split(mod[:, None, :], 6, axis=-1)
        mod_ctx = jax.nn.silu(c_ctx) @ w_ada[l] + b_ada[l]
        csh1, csc1 = mod_ctx[:D], mod_ctx[D:2 * D]

        h = modulate(rmsnorm(x, norm1_g[l]), sh1, sc1)
        proj = h @ w_in[l]
        u_a, v_a, q, k, v, gates = jnp.split(proj, [OFF_VA, OFF_Q, OFF_K, OFF_VB, OFF_GATE], axis=-1)

        hc = modulate(rmsnorm(ctx, norm1_g[l]), csh1, csc1)
        kv_ctx = hc @ w_in[l][:, OFF_K:OFF_GATE]
        k_ctx, v_ctx = jnp.split(kv_ctx.reshape(Bn, -1, 2 * N_KV_HEADS, HEAD_DIM), 2, axis=2)

        y_a = chunk_gmlp(jax.nn.gelu(u_a), jax.nn.gelu(v_a), gmlp_ln_g[l], gmlp_ln_b[l],
                         w_spatial[l], b_spatial[l])

        qh = rope_2d(q.reshape(Bn, L, N_HEADS, HEAD_DIM), row, col)
        kh = rope_2d(k.reshape(Bn, L, N_KV_HEADS, HEAD_DIM), row, col)
        vh = v.reshape(Bn, L, N_KV_HEADS, HEAD_DIM)
        y_b = window_gqa(qh, kh, vh, k_ctx, v_ctx, attn_sink[l])

        g_a, g_b = jnp.split(jax.nn.sigmoid(gates + b_merge_gate[l]), 2, axis=-1)
        merged = g_a * (y_a @ w_proj_a[l]) + g_b * (y_b @ w_proj_b[l])
        x = x + g1 * (merged @ w_out[l])

        h2 = modulate(rmsnorm(x, norm2_g[l]), sh2, sc2)
        x = x + g2 * expert_choice_ffn(h2, w_router[l], w_exp_gate[l], w_exp_up[l], w_exp_down[l])

    return rmsnorm(x, final_g)
```

```python
from contextlib import ExitStack
import numpy as np
import concourse.bass as bass
import concourse.mybir as mybir
from concourse.bass_utils import run_bass_kernel_spmd

F32 = mybir.dt.float32
BF16 = mybir.dt.bfloat16
I32 = mybir.dt.int32
AF = mybir.ActivationFunctionType
ALU = mybir.AluOpType
AX = mybir.AxisListType

D = 1024
L = 4096
NT = 32
CTX = 256
NE = 16
CAP = 512
DE = 2048
EPS = 1e-6
NBIS = 30


class Buf:
    __slots__ = ("name", "w", "r", "sem", "semval", "t")

    def __init__(self, name, t=None):
        self.name = name
        self.w = {}
        self.r = {}
        self.sem = None
        self.t = t

    def __getitem__(self, k):
        return self.t[k]


class Sched:
    COMPUTE = ("pe", "act", "dve", "pool")

    def __init__(self, nc, stack, nsem=84):
        self.nc = nc
        self.eng = {"pe": nc.tensor, "act": nc.scalar, "dve": nc.vector,
                    "pool": nc.gpsimd, "sp": nc.sync}
        self.esem = {}
        self.cnt = {}
        for e in self.COMPUTE:
            self.esem[e] = stack.enter_context(nc.semaphore("e_" + e))
            self.cnt[e] = 0
        self.free = [stack.enter_context(nc.semaphore("d%d" % i)) for i in range(nsem)]
        self.swfree = [self.free.pop() for _ in range(44)]
        self.swsems = set()
        self.semval = {}
        self.known = {e: {} for e in self.eng}
        self.allsems = {}
        self.phase_bufs = []

    def buf(self, name, t=None):
        b = Buf(name, t)
        self.phase_bufs.append(b)
        return b

    def _sem_of(self, b, sw=False):
        if b.sem is None:
            if sw:
                b.sem = self.swfree.pop()
                self.swsems.add(id(b.sem))
            else:
                b.sem = self.free.pop()
            self.semval.setdefault(b.sem, 0)
            self.allsems[id(b.sem)] = b.sem
        return b.sem

    def end_phase(self):
        targets = [(self.esem[e], self.cnt[e]) for e in self.COMPUTE if self.cnt[e] > 0]
        for s in self.allsems.values():
            if self.semval.get(s, 0) > 0:
                targets.append((s, self.semval[s]))
        for e, eng in self.eng.items():
            for s, v in targets:
                if s is self.esem.get(e):
                    continue
                if self.known[e].get(id(s), 0) < v:
                    eng.wait_ge(s, v)
                    self.known[e][id(s)] = v
        for b in self.phase_bufs:
            if b.sem is not None:
                if id(b.sem) not in self.swsems:
                    self.free.append(b.sem)
                b.sem = None
        self.phase_bufs = []

    def _collect(self, e, reads, writes):
        need = {}

        def add(d):
            for k, (s, v) in d.items():
                if k not in need or need[k][1] < v:
                    need[k] = (s, v)
        for b in reads:
            add(b.w)
        for b in writes:
            add(b.w)
            add(b.r)
        out = []
        for k, (s, v) in need.items():
            if e == "pe" and s is self.esem["pe"]:
                continue
            if self.known[e].get(k, 0) >= v:
                continue
            out.append((s, v))
            self.known[e][k] = v
        return out

    def _emit(self, e, fn, waits):
        eng = self.eng[e]
        for s, v in waits[1:]:
            eng.wait_ge(s, v)
        inst = fn(eng)
        if waits:
            inst._wait_ge(waits[0][0], waits[0][1])
        return inst

    def _record(self, ev, reads, writes):
        s, v = ev
        k = id(s)
        for b in reads:
            b.r[k] = (s, v)
        for b in writes:
            b.w = {k: (s, v)}
            b.r = {}

    def op(self, e, fn, reads=(), writes=()):
        waits = self._collect(e, reads, writes)
        inst = self._emit(e, fn, waits)
        self.cnt[e] += 1
        inst.then_inc(self.esem[e], 1)
        self._record((self.esem[e], self.cnt[e]), reads, writes)
        return inst

    def group(self, e, fns, reads=(), writes=()):
        waits = self._collect(e, reads, writes)
        eng = self.eng[e]
        for s, v in waits:
            eng.wait_ge(s, v)
        inst = None
        for fn in fns:
            inst = fn(eng)
        self.cnt[e] += 1
        inst.then_inc(self.esem[e], 1)
        self._record((self.esem[e], self.cnt[e]), reads, writes)

    def dma(self, q, fn, reads=(), writes=(), sembuf=None):
        waits = self._collect(q, reads, writes)
        inst = self._emit(q, fn, waits)
        s = self._sem_of(sembuf, sw=(q == "pool"))
        assert (id(s) in self.swsems) == (q == "pool"), sembuf.name
        self.semval[s] += 16
        inst.then_inc(s, 16)
        self._record((s, self.semval[s]), reads, writes)
        return inst


def _build(debug=False):
    nc = bass.Bass("TRN2", target_bir_lowering=False)
    KIN = "ExternalInput"

    def din(name, shape, dt=F32):
        return nc.dram_tensor(name, list(shape), dt, kind=KIN).ap()

    x_d = din("x", [L, D])
    ctx_d = din("ctx", [CTX, D])
    cc_d = din("cc", [128, 8, 2])
    wada_d = din("w_ada", [D, 6 * D])
    badafp_d = din("bada_fp", [128, 16])
    badabc_d = din("bada_bc", [128, 4 * D])
    n1g_d = din("n1g_fp", [128, 8])
    n2g_d = din("n2g_bc", [128, D])
    fg_d = din("fg_bc", [128, D])
    lng_d = din("lng_bc", [128, D])
    lnb_d = din("lnb_bc", [128, D])
    bmg_d = din("bmg_bc", [128, 2 * D])
    wsT_d = din("wsT", [128, 8, 128])
    wsP_d = din("wsP", [128, 8, 128])
    bs_d = din("bs_pg", [128, 8])
    sink_d = din("sink_bc", [128, 16])
    wr_d = din("wr", [128, 8, NE])
    win_d = din("w_in", [D, 5632])
    wpa_d = din("w_proj_a", [D, D])
    wpb_d = din("w_proj_b", [D, D])
    wout_d = din("w_out", [D, D])
    weg_d = din("w_exp_gate", [NE, D, DE])
    weu_d = din("w_exp_up", [NE, D, DE])
    wed_d = din("w_exp_down", [NE, DE, D])
    cos_d = din("rope_cos", [128, NT, 2, 16])
    sin_d = din("rope_sin", [128, NT, 2, 16])
    mask_d = din("masks", [128, 2, 512])
    ident_d = din("ident", [128, 128])
    iota_d = din("iota", [128, 128])
    pidx_d = din("pidx", [128, 1])
    tri_d = din("tri", [128, 128])
    y_d = nc.dram_tensor("y", [L, D], F32, kind="ExternalOutput").ap()
    skind = "ExternalOutput" if debug else "Internal"
    ma_d = nc.dram_tensor("ma_s", [L, D], F32, kind=skind).ap()
    acc_d = nc.dram_tensor("acc_s", [L, D], F32, kind=skind).ap()
    h2_d = nc.dram_tensor("h2_s", [L, D], BF16, kind=skind).ap()
    aff_d = nc.dram_tensor("aff_s", [L, NE], F32, kind=skind).ap()
    wg_s = nc.dram_tensor("wg_s", [NE, 4, 128, 8, 512], BF16, kind="Internal").ap()
    wu_s = nc.dram_tensor("wu_s", [NE, 4, 128, 8, 512], BF16, kind="Internal").ap()
    wd_s = nc.dram_tensor("wd_s", [NE, 128, 16, D], BF16, kind="Internal").ap()
    g1_s = nc.dram_tensor("g1_s", [128, D], F32, kind="Internal").ap()
    if debug:
        dbg_d = nc.dram_tensor("dbg", [128, 4096], F32, kind="ExternalOutput").ap()

    top = ExitStack()
    with top:
        S = Sched(nc, top)
        pe, act, dve, pool, sp = "pe", "act", "dve", "pool", "sp"

        uniq = [0]

        def sb(stack, name, shape, dt=F32):
            uniq[0] += 1
            t = stack.enter_context(nc.sbuf_tensor("s%d_%s" % (uniq[0], name), list(shape), dt))
            return S.buf(name, t)

        def psbanks(stack):
            return [S.buf("ps%d" % i, stack.enter_context(nc.psum_tensor("ps%d" % i, [128, 512], F32)))
                    for i in range(8)]

        ident_f = sb(top, "ident_f", [128, 128])
        ident_b = sb(top, "ident_b", [128, 128], BF16)
        A1 = sb(top, "A1", [128, 8, 2])
        B1 = sb(top, "B1", [128, 8, 2])
        A2_bc = sb(top, "A2_bc", [128, D])
        B2_bc = sb(top, "B2_bc", [128, D])
        g2_bc = sb(top, "g2_bc", [128, D])
        aff_all = sb(top, "aff_all", [128, NT, NE])
        idx_i = sb(top, "idx_i", [128, NE, 4], I32)
        ones_f = sb(top, "ones_f", [128, 128])
        ones_b = sb(top, "ones_b", [128, 128], BF16)
        epsb = sb(top, "epsb", [128, 1])
        ps = psbanks(top)
        psi = [0]

        psel = [None]
        pcnt = {}

        def nps():
            pool_ = psel[0]
            if pool_ is None:
                b = ps[psi[0] % 8]
                psi[0] += 1
                return b
            k = pcnt.get(pool_, 0)
            pcnt[pool_] = k + 1
            return ps[pool_[k % len(pool_)]]

        ma_b = [S.buf("ma%d" % t) for t in range(NT)]
        acc_b = [S.buf("acc%d" % t) for t in range(NT)]
        h2_b = [S.buf("h2d%d" % t) for t in range(NT)]
        affd_b = [S.buf("affd%d" % t) for t in range(NT)]
        dbg_b = S.buf("dbg")
        g1d_b = S.buf("g1d")
        S.phase_bufs = []

        def load(q, dst, dst_ap, src_ap, reads=()):
            S.dma(q, lambda e: e.dma_start(out=dst_ap, in_=src_ap), reads=reads, writes=[dst], sembuf=dst)

        load(sp, ident_f, ident_f[:], ident_d)
        S.op(dve, lambda e: e.tensor_copy(out=ident_b[:], in_=ident_f[:]), [ident_f], [ident_b])
        S.op(dve, lambda e: e.memset(ones_f[:], 1.0), [], [ones_f])
        S.op(dve, lambda e: e.memset(ones_b[:], 1.0), [], [ones_b])
        S.op(dve, lambda e: e.memset(epsb[:], EPS), [], [epsb])

        def rstd_from_ss(st, ci, co, n=1, sc=8):
            v = st[:, sc:sc + n]; ti = st[:, sc + n:sc + 2 * n].bitcast(I32); tt = st[:, sc + 2 * n:sc + 3 * n]
            y = st[:, co:co + n]
            S.op(dve, lambda e: e.tensor_scalar(out=v, in0=st[:, ci:ci + n], scalar1=1.0 / D, scalar2=EPS,
                                                op0=ALU.mult, op1=ALU.add), [st], [st])
            S.op(dve, lambda e: e.tensor_single_scalar(out=ti, in_=v.bitcast(I32), scalar=1,
                                                       op=ALU.arith_shift_right), [st], [st])
            S.op(dve, lambda e: e.tensor_scalar(out=y.bitcast(I32), in0=ti, scalar1=-1.0,
                                                scalar2=float(0x5f3759df), op0=ALU.mult, op1=ALU.add), [st], [st])
            for _ in range(3):
                S.op(dve, lambda e: e.tensor_tensor(out=tt, in0=y, in1=y, op=ALU.mult), [st], [st])
                S.op(dve, lambda e: e.tensor_tensor(out=tt, in0=tt, in1=v, op=ALU.mult), [st], [st])
                S.op(dve, lambda e: e.tensor_scalar(out=tt, in0=tt, scalar1=-0.5, scalar2=1.5, op0=ALU.mult,
                                                    op1=ALU.add), [st], [st])
                S.op(dve, lambda e: e.tensor_tensor(out=y, in0=y, in1=tt, op=ALU.mult), [st], [st])

        def sumsq(xt, junk, st, ci):
            S.op(dve, lambda e: e.memset(st[:, ci:ci + 1], 0.0), [], [st])
            S.op(act, lambda e: e.activation(out=junk[:], in_=xt[:], func=AF.Square, accum_out=st[:, ci:ci + 1]),
                 [xt], [junk, st])

        def transpose_evac(src, col0, ncols, dtype, evac):
            nblk = ncols // 128
            for k0 in range(0, nblk, 4):
                n = min(4, nblk - k0)
                bank = nps()
                idt = ident_f if dtype == F32 else ident_b
                if dtype == F32:
                    view = bank[:, 0:n * 128]
                else:
                    view = bank[:].bitcast(BF16)[:, 0:n * 128]
                fns = []
                for j in range(n):
                    c = col0 + (k0 + j) * 128
                    fns.append(lambda e, j=j, c=c: e.transpose(view[:, j * 128:(j + 1) * 128],
                                                              src[:, c:c + 128], idt[:]))
                S.group(pe, fns, [src, idt], [bank])
                evac(bank, view, k0, n)
                yield

        def drain(gen):
            for _ in gen:
                pass

        def interleave(gens, pools=None):
            items = [(g, (pools[i] if pools else None)) for i, g in enumerate(gens) if g is not None]
            while items:
                for it in list(items):
                    psel[0] = it[1]
                    try:
                        next(it[0])
                    except StopIteration:
                        items.remove(it)
            psel[0] = None

        def norm1_pro(xt, st, xhat):
            sumsq(xt, xhat, st, 0)
            rstd_from_ss(st, 0, 1)
            S.op(dve, lambda e: e.tensor_scalar(out=xhat[:], in0=xt[:], scalar1=st[:, 1:2], scalar2=None,
                                                op0=ALU.mult), [xt, st], [xhat])

        def norm1_T(xhat, hT, col):
            def evac(bank, view, k0, n):
                for j in range(n):
                    k = k0 + j
                    if k0 == 0:
                        S.op(act, lambda e, j=j, k=k: e.activation(
                            out=hT[:, k, :], in_=view[:, j * 128:(j + 1) * 128], func=AF.Identity,
                            bias=B1[:, k, col:col + 1], scale=A1[:, k, col:col + 1]), [bank, A1, B1], [hT])
                    else:
                        S.op(dve, lambda e, j=j, k=k: e.tensor_scalar(
                            out=hT[:, k, :], in0=view[:, j * 128:(j + 1) * 128], scalar1=A1[:, k, col:col + 1],
                            scalar2=B1[:, k, col:col + 1], op0=ALU.mult, op1=ALU.add), [bank, A1, B1], [hT])
            yield from transpose_evac(xhat, 0, D, F32, evac)

        def norm1_hT(xt, junk, st, xhat, hT, col):
            norm1_pro(xt, st, xhat)
            yield from norm1_T(xhat, hT, col)

        def mm_acc(out_bank, out_ap, pairs, reads):
            n = len(pairs)
            fns = [lambda e, i=i, l=l, r=r: e.matmul(out_ap, l, r, start=(i == 0), stop=(i == n - 1))
                   for i, (l, r) in enumerate(pairs)]
            S.group(pe, fns, reads, [out_bank])

        winv = win_d.rearrange("(k p) f -> p k f", p=128)

        def load_w(dst, dst_ap, src_view, c0, c1):
            for k in range(8):
                S.dma(pool, lambda e, k=k: e.dma_start(out=dst_ap[:, k, :], in_=src_view[:, k, c0:c1]),
                      [], [dst], sembuf=dst)

        wgv = weg_d.rearrange("e (k p) f -> e p k f", p=128)
        wuv = weu_d.rearrange("e (k p) f -> e p k f", p=128)
        wdv = wed_d.rearrange("e (c p) f -> e p c f", p=128)
        castb = S.buf("castb")
        cast_jobs = []
        for ex_ in range(NE):
            for u_ in range(4):
                cast_jobs.append((wg_s[ex_, u_], wgv[ex_, :, :, u_ * 512:(u_ + 1) * 512]))
                cast_jobs.append((wu_s[ex_, u_], wuv[ex_, :, :, u_ * 512:(u_ + 1) * 512]))
            for hh_ in range(2):
                cast_jobs.append((wd_s[ex_][:, hh_ * 8:(hh_ + 1) * 8, :], wdv[ex_, :, hh_ * 8:(hh_ + 1) * 8, :]))
        cast_pos = [0]

        def cast_some(upto):
            upto = min(upto, len(cast_jobs))
            while cast_pos[0] < upto:
                o_, i_ = cast_jobs[cast_pos[0]]
                S.dma(pool, lambda e, o_=o_, i_=i_: e.dma_start(out=o_, in_=i_), [], [], sembuf=castb)
                cast_pos[0] += 1

        p1w = ExitStack()
        w1 = sb(p1w, "w1", [128, 8, 3072], BF16)
        wpa = sb(p1w, "wpa", [128, 8, D], BF16)
        wsT = sb(p1w, "wsT", [128, 8, 128], BF16)
        load_w(w1, w1[:, :, 0:2048], winv, 0, 2048)
        load_w(w1, w1[:, :, 2048:3072], winv, 3584, 4608)
        load_w(wpa, wpa[:], wpa_d.rearrange("(k p) f -> p k f", p=128), 0, D)
        S.dma(pool, lambda e: e.dma_start(out=wsT[:], in_=wsT_d), [], [wsT], sembuf=wsT)
        S.phase_bufs = []

        with ExitStack() as ph:
            cc = sb(ph, "cc", [128, 8, 2])
            sc = sb(ph, "sc", [128, 8, 2])
            sc_rep = sb(ph, "sc_rep", [128, 8, 128])
            bada_fp = sb(ph, "bada_fp", [128, 16])
            bada_bc = sb(ph, "bada_bc", [128, 4 * D])
            n1g = sb(ph, "n1g", [128, 8])
            n2g = sb(ph, "n2g", [128, D])
            modfp = sb(ph, "modfp", [128, 16, 2])
            modbc = sb(ph, "modbc", [128, 4 * D])
            wblk = [sb(ph, "wblk%d" % i, [128, 8, 512]) for i in range(2)]
            load(sp, cc, cc[:], cc_d)
            load(sp, bada_fp, bada_fp[:], badafp_d)
            load(sp, bada_bc, bada_bc[:], badabc_d)
            load(sp, n1g, n1g[:], n1g_d)
            load(sp, n2g, n2g[:], n2g_d)
            S.op(act, lambda e: e.activation(out=sc[:], in_=cc[:], func=AF.Silu), [cc], [sc])
            for k in range(8):
                S.op(dve, lambda e, k=k: e.tensor_copy(out=sc_rep[:, k, :],
                                                       in_=sc[:, k, 0:1].to_broadcast([128, 128])),
                     [sc], [sc_rep])
            wav = wada_d.rearrange("(k p) f -> p k f", p=128)
            for blk in range(12):
                wb = wblk[blk % 2]
                load(sp, wb, wb[:], wav[:, :, blk * 512:(blk + 1) * 512])
                if blk < 4:
                    for j in range(4):
                        cj = blk * 4 + j
                        bank = nps()
                        mm_acc(bank, bank[:, 0:2],
                               [(wb[:, k, j * 128:(j + 1) * 128], sc[:, k, :]) for k in range(8)], [wb, sc])
                        S.op(dve, lambda e, cj=cj, bank=bank: e.tensor_scalar(
                            out=modfp[:, cj, :], in0=bank[:, 0:2], scalar1=bada_fp[:, cj:cj + 1], scalar2=None,
                            op0=ALU.add), [bank, bada_fp], [modfp])
                else:
                    c0 = (blk - 4) * 512
                    bank = nps()
                    mm_acc(bank, bank[:], [(sc_rep[:, k, :], wb[:, k, :]) for k in range(8)], [wb, sc_rep])
                    S.op(dve, lambda e, c0=c0, bank=bank: e.tensor_tensor(
                        out=modbc[:, c0:c0 + 512], in0=bank[:], in1=bada_bc[:, c0:c0 + 512], op=ALU.add),
                        [bank, bada_bc], [modbc])
            S.op(dve, lambda e: e.tensor_scalar(out=A1[:], in0=modfp[:, 8:16, :], scalar1=1.0, scalar2=None,
                                                op0=ALU.add), [modfp], [A1])
            S.op(dve, lambda e: e.tensor_tensor(out=A1[:], in0=A1[:],
                                                in1=n1g[:].unsqueeze(2).to_broadcast([128, 8, 2]), op=ALU.mult),
                 [A1, n1g], [A1])
            S.op(dve, lambda e: e.tensor_copy(out=B1[:], in_=modfp[:, 0:8, :]), [modfp], [B1])
            S.dma(sp, lambda e: e.dma_start(out=g1_s, in_=modbc[:, 0:D]), [modbc], [g1d_b], sembuf=modbc)
            S.op(dve, lambda e: e.tensor_copy(out=B2_bc[:], in_=modbc[:, D:2 * D]), [modbc], [B2_bc])
            S.op(dve, lambda e: e.tensor_scalar(out=A2_bc[:], in0=modbc[:, 2 * D:3 * D], scalar1=1.0, scalar2=None,
                                                op0=ALU.add), [modbc], [A2_bc])
            S.op(dve, lambda e: e.tensor_tensor(out=A2_bc[:], in0=A2_bc[:], in1=n2g[:], op=ALU.mult),
                 [A2_bc, n2g], [A2_bc])
            S.op(dve, lambda e: e.tensor_copy(out=g2_bc[:], in_=modbc[:, 3 * D:4 * D]), [modbc], [g2_bc])
            if debug:
                S.dma(sp, lambda e: e.dma_start(out=dbg_d[:, 0:16], in_=A1[:].rearrange("p a b -> p (a b)")),
                      [A1], [dbg_b], sembuf=A1)
                S.dma(sp, lambda e: e.dma_start(out=dbg_d[:, 16:32], in_=B1[:].rearrange("p a b -> p (a b)")),
                      [B1], [dbg_b], sembuf=B1)
                S.dma(sp, lambda e: e.dma_start(out=dbg_d[:, 1024:2048], in_=A2_bc[:]), [A2_bc], [dbg_b], sembuf=A2_bc)
                S.dma(sp, lambda e: e.dma_start(out=dbg_d[:, 2048:3072], in_=g2_bc[:]), [g2_bc], [dbg_b], sembuf=g2_bc)
            print("P0 sbuf left", nc.sbuf_bytes_remaining)
            S.end_phase()

        with ExitStack() as ph:
            wsP = sb(ph, "wsP", [128, 8, 128])
            rs = sb(ph, "rs", [128, 8])
            bs = sb(ph, "bs", [128, 8])
            lng = sb(ph, "lng", [128, D])
            lnb = sb(ph, "lnb", [128, D])
            biasA = sb(ph, "biasA", [128, D])
            bga = sb(ph, "bga", [128, D])
            xts = [sb(ph, "xt%d" % i, [128, D]) for i in range(3)]
            junk = sb(ph, "junk", [128, D])
            xhats = [sb(ph, "xhat%d" % i, [128, D]) for i in range(2)]
            hTs = [sb(ph, "hT%d" % i, [128, 8, 128], BF16) for i in range(2)]
            us = [sb(ph, "u%d" % i, [128, D]) for i in range(2)]
            vs = sb(ph, "v", [128, D])
            vhs = [sb(ph, "vh%d" % i, [128, D], BF16) for i in range(2)]
            gas = [sb(ph, "ga%d" % i, [128, D]) for i in range(3)]
            t1 = sb(ph, "t1", [128, D])
            yas = [sb(ph, "ya%d" % i, [128, D], BF16) for i in range(2)]
            yaT = sb(ph, "yaT", [128, 8, 128], BF16)
            mas = [sb(ph, "mas%d" % i, [128, D]) for i in range(2)]
            sm = [sb(ph, "sm%d" % i, [128, 16]) for i in range(3)]

            load(sp, wsP, wsP[:], wsP_d)
            load(sp, bs, bs[:], bs_d)
            load(sp, lng, lng[:], lng_d)
            load(sp, lnb, lnb[:], lnb_d)
            load(sp, bga, bga[:], bmg_d[:, 0:D])
            S.op(dve, lambda e: e.tensor_reduce(out=rs[:], in_=wsP[:], axis=AX.X, op=ALU.add), [wsP], [rs])
            for g in range(8):
                S.op(dve, lambda e, g=g: e.tensor_scalar(
                    out=biasA[:, g * 128:(g + 1) * 128], in0=lnb[:, g * 128:(g + 1) * 128],
                    scalar1=rs[:, g:g + 1], scalar2=bs[:, g:g + 1], op0=ALU.mult, op1=ALU.add),
                    [lnb, rs, bs], [biasA])

            def p1_ld(t):
                xt = xts[t % 3]
                load(sp, xt, xt[:], x_d[t * 128:(t + 1) * 128, :])

            def p1_pro(t):
                if t + 1 < NT:
                    p1_ld(t + 1)
                norm1_pro(xts[t % 3], sm[t % 3], xhats[t % 2])

            def p1_A(t):
                hT = hTs[t % 2]; st = sm[t % 3]
                u = us[t % 2]; vh = vhs[t % 2]; ga = gas[t % 3]
                yield from norm1_T(xhats[t % 2], hT, 0)
                if t + 1 < NT:
                    p1_pro(t + 1)
                yield
                S.op(dve, lambda e: e.memset(st[:, 2:4], 0.0), [], [st])

                def grp(gi):
                    bank = nps()
                    mm_acc(bank, bank[:], [(hT[:, k, :], w1[:, k, gi * 512:(gi + 1) * 512]) for k in range(8)],
                           [hT, w1])
                    h = gi % 2
                    if gi < 2:
                        S.op(act, lambda e, bank=bank, h=h: e.activation(
                            out=u[:, h * 512:(h + 1) * 512], in_=bank[:], func=AF.Gelu_apprx_tanh), [bank], [u])
                    elif gi < 4:
                        S.op(act, lambda e, bank=bank, h=h: e.activation(
                            out=vs[:, h * 512:(h + 1) * 512], in_=bank[:], func=AF.Gelu_apprx_tanh,
                            accum_out=st[:, 2 + h:3 + h]), [bank], [vs, st])
                    else:
                        S.op(dve, lambda e, bank=bank, h=h: e.tensor_tensor(
                            out=ga[:, h * 512:(h + 1) * 512], in0=bank[:], in1=bga[:, h * 512:(h + 1) * 512],
                            op=ALU.add), [bank, bga], [ga])
                        S.op(act, lambda e, h=h: e.activation(
                            out=ga[:, h * 512:(h + 1) * 512], in_=ga[:, h * 512:(h + 1) * 512], func=AF.Tanh,
                            scale=0.5), [ga], [ga])

                grp(2)
                yield
                grp(3)
                S.op(dve, lambda e: e.tensor_tensor(out=st[:, 4:5], in0=st[:, 2:3], in1=st[:, 3:4], op=ALU.add),
                     [st], [st])
                S.op(dve, lambda e: e.tensor_scalar(out=st[:, 4:5], in0=st[:, 4:5], scalar1=-1.0 / D, scalar2=None,
                                                    op0=ALU.mult), [st], [st])
                S.op(dve, lambda e: e.memset(st[:, 5:6], 0.0), [], [st])
                S.op(act, lambda e: e.activation(out=junk[:], in_=vs[:], func=AF.Square, bias=st[:, 4:5],
                                                 accum_out=st[:, 5:6]), [vs, st], [junk, st])
                rstd_from_ss(st, 5, 6)
                S.op(dve, lambda e: e.tensor_scalar(out=vh[:], in0=vs[:], scalar1=st[:, 4:5], scalar2=st[:, 6:7],
                                                    op0=ALU.add, op1=ALU.mult), [vs, st], [vh])
                yield
                for gi in (0, 1, 4, 5):
                    grp(gi)
                    yield

            def p1_B(t):
                u = us[t % 2]; vh = vhs[t % 2]; ya = yas[t % 2]
                for h in range(2):
                    bank = nps()
                    fns = [lambda e, g=g, bank=bank: e.matmul(
                        bank[:, (g % 4) * 128:(g % 4 + 1) * 128], wsT[:, g, :], vh[:, g * 128:(g + 1) * 128],
                        start=True, stop=True) for g in range(h * 4, h * 4 + 4)]
                    S.group(pe, fns, [wsT, vh], [bank])
                    sl = slice(h * 512, (h + 1) * 512)
                    S.op(dve, lambda e, bank=bank, sl=sl: e.tensor_tensor(out=t1[:, sl], in0=bank[:], in1=lng[:, sl],
                                                                         op=ALU.mult), [bank, lng], [t1])
                    S.op(dve, lambda e, sl=sl: e.tensor_tensor(out=t1[:, sl], in0=t1[:, sl], in1=biasA[:, sl],
                                                               op=ALU.add), [t1, biasA], [t1])
                    S.op(dve, lambda e, sl=sl: e.tensor_tensor(out=ya[:, sl], in0=t1[:, sl], in1=u[:, sl],
                                                              op=ALU.mult), [t1, u], [ya])
                    yield
                    yield

            def p1_C(t):
                ga = gas[t % 3]; ma = mas[t % 2]; ya = yas[t % 2]

                def evac(bank, view, k0, n):
                    if k0 == 0:
                        S.op(act, lambda e: e.activation(out=yaT[:, k0:k0 + n, :].rearrange("p a b -> p (a b)"),
                                                         in_=view, func=AF.Copy), [bank], [yaT])
                    else:
                        S.op(dve, lambda e: e.tensor_copy(out=yaT[:, k0:k0 + n, :].rearrange("p a b -> p (a b)"),
                                                          in_=view), [bank], [yaT])
                yield from transpose_evac(ya, 0, D, BF16, evac)
                yield
                for h in range(2):
                    bank = nps()
                    sl = slice(h * 512, (h + 1) * 512)
                    mm_acc(bank, bank[:], [(yaT[:, k, :], wpa[:, k, sl]) for k in range(8)], [yaT, wpa])
                    S.op(dve, lambda e, bank=bank, sl=sl: e.scalar_tensor_tensor(
                        out=ma[:, sl], in0=ga[:, sl], scalar=1.0, in1=bank[:], op0=ALU.add, op1=ALU.mult),
                        [bank, ga], [ma])
                    yield
                S.dma(sp, lambda e: e.dma_start(out=ma_d[t * 128:(t + 1) * 128, :], in_=ma[:]),
                      [ma], [ma_b[t]], sembuf=ma)

            print("P1 sbuf left", nc.sbuf_bytes_remaining)
            p1_ld(0)
            p1_pro(0)
            for t in range(NT + 2):
                cast_some(int(len(cast_jobs) * (t + 1) / 66.0))
                interleave([p1_A(t) if t < NT else None, p1_B(t - 1) if 0 <= t - 1 < NT else None,
                            p1_C(t - 2) if 0 <= t - 2 < NT else None],
                           pools=[(0, 1, 2, 3), (4, 5), (6, 7)])
            S.end_phase()
        p1w.close()

        with ExitStack() as ph:
            w2 = sb(ph, "w2", [128, 8, 2560], BF16)
            wpb = sb(ph, "wpb", [128, 8, D], BF16)
            wo = sb(ph, "wo", [128, 8, D], BF16)
            cosT = sb(ph, "cosT", [128, NT, 2, 16])
            sinT = sb(ph, "sinT", [128, NT, 2, 16])
            mask = sb(ph, "mask", [128, 2, 512], BF16)
            bgb = sb(ph, "bgb", [128, D])
            esink = sb(ph, "esink", [128, 16])
            wr = sb(ph, "wr", [128, 8, NE])
            xts = [sb(ph, "xt%d" % i, [128, D]) for i in range(2)]
            xres = sb(ph, "xres", [128, D])
            xhats = [sb(ph, "xhat%d" % i, [128, D]) for i in range(2)]
            hTs = [sb(ph, "hT%d" % i, [128, 8, 128], BF16) for i in range(4)]
            qsb = sb(ph, "qsb", [128, 1280])
            rt = [sb(ph, "rt%d" % i, [128, 256]) for i in range(4)]
            qrot = sb(ph, "qrot", [128, 1280], BF16)
            QTs = [sb(ph, "QT%d" % i, [64, 16, 128], BF16) for i in range(3)]
            KTs = [sb(ph, "KT%d" % i, [64, 4, 128], BF16) for i in range(4)]
            KTc = [sb(ph, "KTc%d" % i, [64, 4, 128], BF16) for i in range(2)]
            Vs = [sb(ph, "V%d" % i, [128, 4, 65], BF16) for i in range(4)]
            Vc = [sb(ph, "Vc%d" % i, [128, 4, 65], BF16) for i in range(2)]
            gb = sb(ph, "gb", [128, D])
            PTs = [sb(ph, "PT%d" % i, [128, 512], BF16) for i in range(10)]
            den = sb(ph, "den", [128, 8])
            yb = sb(ph, "yb", [128, D], BF16)
            ybTs = [sb(ph, "ybT%d" % i, [128, 8, 128], BF16) for i in range(2)]
            mat = sb(ph, "mat0", [128, D])
            mgb = sb(ph, "mgb", [128, D], BF16)
            mgT = sb(ph, "mgT", [128, 8, 128], BF16)
            h2fs = [sb(ph, "h2f%d" % i, [128, D]) for i in range(2)]
            g1_bc = h2fs[1]
            load(sp, g1_bc, g1_bc[:], g1_s, reads=[g1d_b])
            wtmp = xts
            h2b = sb(ph, "h2b", [128, D], BF16)
            h2T = sb(ph, "h2T", [128, 8, 128])
            smA = [sb(ph, "smA%d" % i, [128, 16]) for i in range(3)]
            smBs = [sb(ph, "smB%d" % i, [128, 16]) for i in range(2)]
            lg = sb(ph, "lg", [128, NE])
            lg2 = sb(ph, "lg2", [128, NE])

            load_w(w2, w2[:, :, 0:1536], winv, 2048, 3584)
            load_w(w2, w2[:, :, 1536:2560], winv, 4608, 5632)
            load_w(wpb, wpb[:], wpb_d.rearrange("(k p) f -> p k f", p=128), 0, D)
            wov = wout_d.rearrange("(k p) f -> p k f", p=128)
            for k in range(8):
                wt_ = wtmp[k % 2]
                load(sp, wt_, wt_[:], wov[:, k, :])
                S.op(dve, lambda e, k=k, wt_=wt_: e.scalar_tensor_tensor(
                    out=wo[:, k, :], in0=wt_[:], scalar=0.5, in1=g1_bc[:], op0=ALU.mult, op1=ALU.mult),
                    [wt_, g1_bc], [wo])
            load(sp, cosT, cosT[:], cos_d)
            load(sp, sinT, sinT[:], sin_d)
            S.dma(pool, lambda e: e.dma_start(out=mask[:], in_=mask_d), [], [mask], sembuf=mask)
            load(sp, bgb, bgb[:], bmg_d[:, D:2 * D])
            load(sp, esink, esink[:], sink_d)
            S.op(act, lambda e: e.activation(out=esink[:], in_=esink[:], func=AF.Exp), [esink], [esink])
            load(sp, wr, wr[:], wr_d)
            for v_ in Vs + Vc:
                S.op(dve, lambda e, v_=v_: e.memset(v_[:], 1.0), [], [v_])

            def rope(src, c0, nh, t, dst):
                sv = src[:, c0:c0 + nh * 64].rearrange("p (h a f i) -> p h a f i", h=nh, a=2, f=2, i=16)
                dv = dst[:, c0:c0 + nh * 64].rearrange("p (h a f i) -> p h a f i", h=nh, a=2, f=2, i=16)
                x1 = sv[:, :, :, 0, :]
                x2 = sv[:, :, :, 1, :]
                cb = cosT[:, t].unsqueeze(1).to_broadcast([128, nh, 2, 16])
                sn = sinT[:, t].unsqueeze(1).to_broadcast([128, nh, 2, 16])
                n = nh * 32
                tv = [r_[:, 0:n].rearrange("p (h a i) -> p h a i", h=nh, a=2, i=16) for r_ in rt]
                S.op(dve, lambda e: e.tensor_tensor(out=tv[0], in0=x1, in1=cb, op=ALU.mult), [src, cosT], [rt[0]])
                S.op(dve, lambda e: e.tensor_tensor(out=tv[1], in0=x2, in1=sn, op=ALU.mult), [src, sinT], [rt[1]])
                S.op(dve, lambda e: e.tensor_tensor(out=dv[:, :, :, 0, :], in0=tv[0], in1=tv[1], op=ALU.subtract),
                     [rt[0], rt[1]], [dst])
                S.op(dve, lambda e: e.tensor_tensor(out=tv[2], in0=x1, in1=sn, op=ALU.mult), [src, sinT], [rt[2]])
                S.op(dve, lambda e: e.tensor_tensor(out=tv[3], in0=x2, in1=cb, op=ALU.mult), [src, cosT], [rt[3]])
                S.op(dve, lambda e: e.tensor_tensor(out=dv[:, :, :, 1, :], in0=tv[2], in1=tv[3], op=ALU.add),
                     [rt[2], rt[3]], [dst])

            def kv_part(hT_, t, KT, V, do_rope):
                bank = nps()
                mm_acc(bank, bank[:], [(hT_[:, k, :], w2[:, k, 1024:1536]) for k in range(8)], [hT_, w2])
                S.op(act, lambda e: e.activation(out=qsb[:, 1024:1280], in_=bank[:, 0:256], func=AF.Copy),
                     [bank], [qsb])
                S.op(act, lambda e: e.activation(out=V[:, :, 0:64],
                                                 in_=bank[:, 256:512].rearrange("p (h d) -> p h d", h=4),
                                                 func=AF.Copy), [bank], [V])
                if do_rope:
                    rope(qsb, 1024, 4, t, qrot)
                else:
                    S.op(dve, lambda e: e.tensor_copy(out=qrot[:, 1024:1280], in_=qsb[:, 1024:1280]), [qsb], [qrot])
                yield
                yield
                bank2 = nps()
                vw = bank2[:].bitcast(BF16)
                fns = [lambda e, h=h: e.transpose(vw[0:64, h * 128:(h + 1) * 128],
                                                  qrot[:, 1024 + h * 64:1024 + (h + 1) * 64], ident_b[:])
                       for h in range(4)]
                S.group(pe, fns, [qrot, ident_b], [bank2])
                S.op(act, lambda e: e.activation(out=KT[:].rearrange("p a b -> p (a b)"), in_=vw[0:64, 0:512],
                                                 func=AF.Copy), [bank2], [KT])
                yield

            for ci in range(2):
                xt = xts[ci]
                load(sp, xt, xt[:], ctx_d[ci * 128:(ci + 1) * 128, :])
                drain(norm1_hT(xt, None, smA[ci], xhats[0], hTs[0], 1))
                drain(kv_part(hTs[0], 0, KTc[ci], Vc[ci], False))

            def p2_pro(t):
                xt = xts[t % 2]
                load(sp, xt, xt[:], x_d[t * 128:(t + 1) * 128, :])
                norm1_pro(xt, smA[t % 3], xhats[t % 2])

            def p2_A(t):
                QT = QTs[t % 3]; hT = hTs[t % 4]
                yield from norm1_T(xhats[t % 2], hT, 0)
                if t + 1 < NT:
                    p2_pro(t + 1)
                yield
                for h in range(2):
                    bank = nps()
                    mm_acc(bank, bank[:], [(hT[:, k, :], w2[:, k, h * 512:(h + 1) * 512]) for k in range(8)], [hT, w2])
                    S.op(act, lambda e, bank=bank, h=h: e.activation(out=qsb[:, h * 512:(h + 1) * 512], in_=bank[:],
                                                                     func=AF.Copy), [bank], [qsb])
                    yield
                rope(qsb, 0, 8, t, qrot)
                rope(qsb, 512, 8, t, qrot)
                yield
                yield from kv_part(hT, t, KTs[t % 4], Vs[t % 4], True)
                yield
                yield
                yield
                for h4 in range(4):
                    bank = nps()
                    vw = bank[:].bitcast(BF16)
                    fns = [lambda e, j=j, vw=vw: e.transpose(vw[0:64, j * 128:(j + 1) * 128],
                                                             qrot[:, (h4 * 4 + j) * 64:(h4 * 4 + j + 1) * 64], ident_b[:])
                           for j in range(4)]
                    S.group(pe, fns, [qrot, ident_b], [bank])
                    if h4 % 2 == 0:
                        S.op(act, lambda e, vw=vw: e.activation(
                            out=QT[:, h4 * 4:h4 * 4 + 4, :].rearrange("p a b -> p (a b)"), in_=vw[0:64, 0:512],
                            func=AF.Copy), [bank], [QT])
                    else:
                        S.op(dve, lambda e, vw=vw: e.tensor_copy(
                            out=QT[:, h4 * 4:h4 * 4 + 4, :].rearrange("p a b -> p (a b)"), in_=vw[0:64, 0:512]),
                            [bank], [QT])
                    yield

            def p2_B1(t):
                QT = QTs[t % 3]; ybT = ybTs[t % 2]
                keys = []
                if t > 0:
                    keys.append((KTs[(t - 1) % 4], Vs[(t - 1) % 4], 0))
                keys.append((KTs[t % 4], Vs[t % 4], None))
                if t < NT - 1:
                    keys.append((KTs[(t + 1) % 4], Vs[(t + 1) % 4], 1))
                keys.append((KTc[0], Vc[0], None))
                keys.append((KTc[1], Vc[1], None))
                nk = len(keys)

                def qk(kvh):
                    pts = [PTs[(kvh % 2) * 5 + i] for i in range(nk)]
                    for i, (KT, V, mi) in enumerate(keys):
                        bank = nps()
                        S.group(pe, [lambda e, KT=KT, bank=bank: e.matmul(
                            bank[:], KT[:, kvh, :], QT[:, kvh * 4:kvh * 4 + 4, :], start=True, stop=True)],
                            [KT, QT], [bank])
                        S.op(act, lambda e, bank=bank, i=i: e.activation(out=pts[i][:], in_=bank[:], func=AF.Exp,
                                                                         scale=0.125), [bank], [pts[i]])
                        if mi is not None:
                            S.op(dve, lambda e, i=i, mi=mi: e.tensor_tensor(out=pts[i][:], in0=pts[i][:],
                                                                           in1=mask[:, mi, :], op=ALU.mult),
                                 [pts[i], mask], [pts[i]])
                        if i % 2 == 1:
                            yield
                    yield

                def pv(kvh):
                    pts = [PTs[(kvh % 2) * 5 + i] for i in range(nk)]
                    obank = nps()
                    ov = obank[:, 0:260].rearrange("p (r d) -> p r d", r=4)
                    fns = []
                    for r in range(4):
                        for i, (KT, V, mi) in enumerate(keys):
                            fns.append(lambda e, r=r, i=i, V=V: e.matmul(
                                ov[:, r, :], pts[i][:, r * 128:(r + 1) * 128], V[:, kvh, :],
                                start=(i == 0), stop=(i == nk - 1)))
                    S.group(pe, fns, pts + [k_[1] for k_ in keys], [obank])
                    S.op(dve, lambda e, ov=ov: e.tensor_tensor(out=den[:, 0:4], in0=ov[:, :, 64],
                                                               in1=esink[:, kvh * 4:kvh * 4 + 4], op=ALU.add),
                         [obank, esink], [den])
                    S.op(dve, lambda e: e.reciprocal(out=den[:, 4:8], in_=den[:, 0:4]), [den], [den])
                    S.op(dve, lambda e, ov=ov: e.tensor_tensor(
                        out=yb[:, kvh * 256:(kvh + 1) * 256].rearrange("p (r d) -> p r d", r=4), in0=ov[:, :, 0:64],
                        in1=den[:, 4:8].unsqueeze(2).to_broadcast([128, 4, 64]), op=ALU.mult), [obank, den], [yb])
                    yield

                yield from qk(0)
                for kvh in range(4):
                    if kvh + 1 < 4:
                        yield from qk(kvh + 1)
                    yield from pv(kvh)

                def evac(bank, view, k0, n):
                    if k0 == 0:
                        S.op(act, lambda e: e.activation(out=ybT[:, k0:k0 + n, :].rearrange("p a b -> p (a b)"),
                                                         in_=view, func=AF.Copy), [bank], [ybT])
                    else:
                        S.op(dve, lambda e: e.tensor_copy(out=ybT[:, k0:k0 + n, :].rearrange("p a b -> p (a b)"),
                                                          in_=view), [bank], [ybT])
                yield from transpose_evac(yb, 0, D, BF16, evac)

            def p2_B2(t):
                hT = hTs[t % 4]; ybT = ybTs[t % 2]; st = smBs[t % 2]; h2f = h2fs[t % 2]; mg = h2f
                x1 = xres
                load(sp, xres, xres[:], x_d[t * 128:(t + 1) * 128, :])
                load(sp, mat, mat[:], ma_d[t * 128:(t + 1) * 128, :], reads=[ma_b[t]])
                for h in range(2):
                    bank = nps()
                    mm_acc(bank, bank[:], [(hT[:, k, :], w2[:, k, 1536 + h * 512:1536 + (h + 1) * 512]) for k in range(8)],
                           [hT, w2])
                    sl = slice(h * 512, (h + 1) * 512)
                    S.op(dve, lambda e, bank=bank, sl=sl: e.tensor_tensor(out=gb[:, sl], in0=bank[:], in1=bgb[:, sl],
                                                                         op=ALU.add), [bank, bgb], [gb])
                    S.op(act, lambda e, sl=sl: e.activation(out=gb[:, sl], in_=gb[:, sl], func=AF.Tanh, scale=0.5),
                         [gb], [gb])
                    yield
                for h in range(2):
                    bank = nps()
                    sl = slice(h * 512, (h + 1) * 512)
                    mm_acc(bank, bank[:], [(ybT[:, k, :], wpb[:, k, sl]) for k in range(8)], [ybT, wpb])
                    S.op(dve, lambda e, bank=bank, sl=sl: e.scalar_tensor_tensor(
                        out=mg[:, sl], in0=gb[:, sl], scalar=1.0, in1=bank[:], op0=ALU.add, op1=ALU.mult),
                        [bank, gb], [mg])
                    S.op(dve, lambda e, sl=sl: e.tensor_tensor(out=mgb[:, sl], in0=mg[:, sl], in1=mat[:, sl],
                                                               op=ALU.add), [mg, mat], [mgb])
                    yield

                def evac2(bank, view, k0, n):
                    if k0 == 0:
                        S.op(act, lambda e: e.activation(out=mgT[:, k0:k0 + n, :].rearrange("p a b -> p (a b)"),
                                                         in_=view, func=AF.Copy), [bank], [mgT])
                    else:
                        S.op(dve, lambda e: e.tensor_copy(out=mgT[:, k0:k0 + n, :].rearrange("p a b -> p (a b)"),
                                                          in_=view), [bank], [mgT])
                yield from transpose_evac(mgb, 0, D, BF16, evac2)
                for h in range(2):
                    bank = nps()
                    sl = slice(h * 512, (h + 1) * 512)
                    mm_acc(bank, bank[:], [(mgT[:, k, :], wo[:, k, sl]) for k in range(8)], [mgT, wo])
                    S.op(dve, lambda e, bank=bank, sl=sl: e.tensor_tensor(out=x1[:, sl], in0=bank[:], in1=x1[:, sl],
                                                                         op=ALU.add), [bank, x1], [x1])
                    yield
                S.dma(sp, lambda e: e.dma_start(out=acc_d[t * 128:(t + 1) * 128, :], in_=x1[:]),
                      [x1], [acc_b[t]], sembuf=x1)
                sumsq(x1, h2f, st, 2)
                rstd_from_ss(st, 2, 3)
                S.op(dve, lambda e: e.scalar_tensor_tensor(out=h2f[:], in0=x1[:], scalar=st[:, 3:4], in1=A2_bc[:],
                                                           op0=ALU.mult, op1=ALU.mult), [x1, st, A2_bc], [h2f])
                S.op(dve, lambda e: e.tensor_tensor(out=h2f[:], in0=h2f[:], in1=B2_bc[:], op=ALU.add),
                     [h2f, B2_bc], [h2f])
                S.op(dve, lambda e: e.tensor_copy(out=h2b[:], in_=h2f[:]), [h2f], [h2b])
                S.dma(sp, lambda e: e.dma_start(out=h2_d[t * 128:(t + 1) * 128, :], in_=h2b[:]),
                      [h2b], [h2_b[t]], sembuf=h2b)
                yield

            def p2_B3(t):
                st = smBs[t % 2]; h2f = h2fs[t % 2]

                def evac3(bank, view, k0, n):
                    S.op(act, lambda e: e.activation(out=h2T[:, k0:k0 + n, :].rearrange("p a b -> p (a b)"),
                                                     in_=view, func=AF.Copy), [bank], [h2T])
                yield from transpose_evac(h2f, 0, D, F32, evac3)
                bank = nps()
                mm_acc(bank, bank[:, 0:NE], [(h2T[:, k, :], wr[:, k, :]) for k in range(8)], [h2T, wr])
                S.op(dve, lambda e: e.tensor_reduce(out=st[:, 4:5], in_=bank[:, 0:NE], axis=AX.X, op=ALU.max),
                     [bank], [st])
                S.op(dve, lambda e: e.tensor_scalar(out=st[:, 4:5], in0=st[:, 4:5], scalar1=-1.0, scalar2=None,
                                                    op0=ALU.mult), [st], [st])
                S.op(dve, lambda e: e.memset(st[:, 5:6], 0.0), [], [st])
                S.op(act, lambda e: e.activation(out=lg[:], in_=bank[:, 0:NE], func=AF.Exp, bias=st[:, 4:5],
                                                 accum_out=st[:, 5:6]), [bank, st], [lg, st])
                S.op(dve, lambda e: e.reciprocal(out=st[:, 6:7], in_=st[:, 5:6]), [st], [st])
                S.op(dve, lambda e: e.tensor_scalar(out=aff_all[:, t, :], in0=lg[:], scalar1=st[:, 6:7], scalar2=None,
                                                    op0=ALU.mult), [lg, st], [aff_all])
                S.op(dve, lambda e: e.tensor_copy(out=lg2[:], in_=aff_all[:, t, :]), [aff_all], [lg2])
                S.dma(sp, lambda e: e.dma_start(out=aff_d[t * 128:(t + 1) * 128, :], in_=lg2[:]),
                      [lg2], [affd_b[t]], sembuf=lg2)

            print("P2 sbuf left", nc.sbuf_bytes_remaining)
            p2_pro(0)
            for i in range(NT + 4):
                cast_some(int(len(cast_jobs) * (34 + i) / 66.0))
                interleave([p2_A(i) if i < NT else None,
                            p2_B1(i - 2) if 0 <= i - 2 < NT else None,
                            p2_B2(i - 3) if 0 <= i - 3 < NT else None,
                            p2_B3(i - 4) if 0 <= i - 4 < NT else None],
                           pools=[(0, 1), (2, 3, 4, 5), (6, 7), (6, 7)])
            cast_some(len(cast_jobs))
            S.end_phase()

        with ExitStack() as ph:
            lo = sb(ph, "lo", [128, NE]); hi = sb(ph, "hi", [128, NE]); mid = sb(ph, "mid", [128, NE])
            cmp_ = sb(ph, "cmp", [128, NT, NE]); cnt = sb(ph, "cnt", [128, NE])
            pred = sb(ph, "pred", [128, NE]); npred = sb(ph, "npred", [128, NE]); tq = sb(ph, "tq", [128, NE])
            selb = sb(ph, "selb", [128, NT, NE], BF16)
            csf = sb(ph, "csf", [128, NT, NE]); csb = sb(ph, "csb", [128, NT, NE], BF16)
            tri_f = sb(ph, "tri_f", [128, 128]); tri_b = sb(ph, "tri_b", [128, 128], BF16)
            iota = sb(ph, "iota", [128, 128]); pidx = sb(ph, "pidx", [128, 1])
            pos = sb(ph, "pos", [128, NT, NE]); posi = sb(ph, "posi", [128, NT, NE], I32)
            clo_i = sb(ph, "clo_i", [128, NT, NE], I32); chi_i = sb(ph, "chi_i", [128, NT, NE], I32)
            clo = sb(ph, "clo", [128, NT, NE]); chi = sb(ph, "chi", [128, NT, NE])
            As = [sb(ph, "A%d" % i, [128, NE, 128], BF16) for i in range(3)]
            Bs = [sb(ph, "B%d" % i, [128, NE, 4], BF16) for i in range(2)]
            Rs = [sb(ph, "R%d" % i, [128, NE, 8], BF16) for i in range(2)]
            idxf = sb(ph, "idxf", [128, NE, 4])
            load(sp, tri_f, tri_f[:], tri_d)
            load(sp, iota, iota[:], iota_d)
            load(sp, pidx, pidx[:], pidx_d)
            S.op(dve, lambda e: e.tensor_copy(out=tri_b[:], in_=tri_f[:]), [tri_f], [tri_b])
            S.op(dve, lambda e: e.memset(lo[:], 0.0), [], [lo])
            affv = aff_all[:]
            for it in range(NBIS):
                wk = 2.0 ** (-(it + 1))
                S.op(dve, lambda e, wk=wk: e.tensor_scalar(out=mid[:], in0=lo[:], scalar1=wk, scalar2=None,
                                                           op0=ALU.add), [lo], [mid])
                S.op(dve, lambda e: e.tensor_tensor(out=cmp_[:], in0=affv,
                                                    in1=mid[:].unsqueeze(1).to_broadcast([128, NT, NE]), op=ALU.is_ge),
                     [aff_all, mid], [cmp_])
                S.op(dve, lambda e: e.tensor_reduce(out=cnt[:], in_=cmp_[:].rearrange("p t e -> p e t"), axis=AX.X,
                                                    op=ALU.add), [cmp_], [cnt])
                bank = nps()
                mm_acc(bank, bank[:, 0:NE], [(ones_f[:], cnt[:])], [ones_f, cnt])
                S.op(dve, lambda e, bank=bank: e.tensor_scalar(out=pred[:], in0=bank[:, 0:NE], scalar1=float(CAP) - 0.5,
                                                               scalar2=None, op0=ALU.is_ge), [bank], [pred])
                S.op(dve, lambda e: e.tensor_tensor(out=tq[:], in0=mid[:], in1=pred[:], op=ALU.mult), [mid, pred], [tq])
                S.op(dve, lambda e: e.tensor_tensor(out=lo[:], in0=lo[:], in1=tq[:], op=ALU.max), [lo, tq], [lo])
            S.op(dve, lambda e: e.tensor_tensor(out=cmp_[:], in0=affv, in1=lo[:].unsqueeze(1).to_broadcast([128, NT, NE]),
                                                op=ALU.is_ge), [aff_all, lo], [cmp_])
            S.op(dve, lambda e: e.tensor_copy(out=selb[:], in_=cmp_[:]), [cmp_], [selb])
            S.op(dve, lambda e: e.memset(csf[:, 0, :], 0.0), [], [csf])
            for t in range(1, NT):
                S.op(dve, lambda e, t=t: e.tensor_tensor(out=csf[:, t, :], in0=csf[:, t - 1, :], in1=cmp_[:, t - 1, :],
                                                         op=ALU.add), [csf, cmp_], [csf])
            S.op(dve, lambda e: e.tensor_copy(out=csb[:], in_=csf[:]), [csf], [csb])
            bank = nps()
            fl = "p t e -> p (t e)"
            S.group(pe, [lambda e: e.matmul(bank[:], tri_b[:], selb[:].rearrange(fl), start=True, stop=False),
                         lambda e: e.matmul(bank[:], ones_b[:], csb[:].rearrange(fl), start=False, stop=True)],
                    [tri_b, selb, ones_b, csb], [bank])
            S.op(dve, lambda e: e.tensor_scalar(out=cmp_[:], in0=cmp_[:], scalar1=-8192.0, scalar2=8192.0, op0=ALU.mult,
                                                op1=ALU.add), [cmp_], [cmp_])
            S.op(dve, lambda e: e.tensor_tensor(out=pos[:].rearrange(fl), in0=bank[:], in1=cmp_[:].rearrange(fl),
                                                op=ALU.add), [bank, cmp_], [pos])
            S.op(dve, lambda e: e.tensor_copy(out=posi[:], in_=pos[:]), [pos], [posi])
            S.op(dve, lambda e: e.tensor_single_scalar(out=clo_i[:], in_=posi[:], scalar=7, op=ALU.arith_shift_right),
                 [posi], [clo_i])
            S.op(dve, lambda e: e.tensor_single_scalar(out=chi_i[:], in_=posi[:], scalar=127, op=ALU.bitwise_and),
                 [posi], [chi_i])
            S.op(dve, lambda e: e.tensor_copy(out=clo[:], in_=clo_i[:]), [clo_i], [clo])
            S.op(dve, lambda e: e.tensor_copy(out=chi[:], in_=chi_i[:]), [chi_i], [chi])
            idxs = sb(ph, "idxs", [128, NE, 8])
            S.op(dve, lambda e: e.memset(idxs[:], 0.0), [], [idxs])
            for t in range(NT):
                A = As[t % 3]; B = Bs[t % 2]; R = Rs[t % 2]
                S.op(dve, lambda e, t=t, A=A: e.tensor_tensor(
                    out=A[:], in0=chi[:, t, :].unsqueeze(2).to_broadcast([128, NE, 128]),
                    in1=iota[:].unsqueeze(1).to_broadcast([128, NE, 128]), op=ALU.is_equal), [chi, iota], [A])
                S.op(dve, lambda e, t=t, B=B: e.tensor_tensor(
                    out=B[:], in0=clo[:, t, :].unsqueeze(2).to_broadcast([128, NE, 4]),
                    in1=iota[:, 0:4].unsqueeze(1).to_broadcast([128, NE, 4]), op=ALU.is_equal), [clo, iota], [B])
                S.op(dve, lambda e, t=t, B=B, R=R: e.tensor_scalar(out=R[:, :, 0:4], in0=B[:], scalar1=float(t),
                                                                   scalar2=None, op0=ALU.mult), [B], [R])
                S.op(dve, lambda e, B=B, R=R: e.tensor_scalar(out=R[:, :, 4:8], in0=B[:], scalar1=pidx[:, 0:1],
                                                              scalar2=None, op0=ALU.mult), [B, pidx], [R])
                ibank = nps()
                iv = ibank[:, 0:NE * 8].rearrange("p (e c) -> p e c", e=NE)
                fns = [lambda e, ex=ex, A=A, R=R, iv=iv: e.matmul(iv[:, ex, :], A[:, ex, :], R[:, ex, :],
                                                                  start=True, stop=True)
                       for ex in range(NE)]
                S.group(pe, fns, [A, R], [ibank])
                S.op(dve, lambda e, iv=iv: e.tensor_tensor(out=idxs[:], in0=iv, in1=idxs[:], op=ALU.add),
                     [ibank, idxs], [idxs])
            S.op(dve, lambda e: e.scalar_tensor_tensor(out=idxf[:], in0=idxs[:, :, 0:4], scalar=128.0, in1=idxs[:, :, 4:8],
                                                       op0=ALU.mult, op1=ALU.add), [idxs], [idxf])
            S.op(dve, lambda e: e.tensor_copy(out=idx_i[:], in_=idxf[:]), [idxf], [idx_i])
            if debug:
                S.dma(sp, lambda e: e.dma_start(out=dbg_d[:, 32:48], in_=lo[:]), [lo], [dbg_b], sembuf=lo)
                S.dma(sp, lambda e: e.dma_start(out=dbg_d[:, 64:128], in_=idxf[:].rearrange("p a b -> p (a b)")),
                      [idxf], [dbg_b], sembuf=idxf)
            S.end_phase()

        with ExitStack() as ph:
            wgu = [sb(ph, "wgu%d" % i, [128, 2, 8, 512], BF16) for i in range(4)]
            wd = [sb(ph, "wd%d" % i, [128, 16, D], BF16) for i in range(2)]
            xe = [[sb(ph, "xe%d_%d" % (i, c), [128, D], BF16) for c in range(4)] for i in range(2)]
            affg = [[sb(ph, "affg%d_%d" % (i, c), [128, NE]) for c in range(4)] for i in range(3)]
            xeT = sb(ph, "xeT", [128, 8, 512], BF16)
            sg = [sb(ph, "sg%d" % i, [128, 512]) for i in range(2)]
            hTm = sb(ph, "hTm", [128, 16, 512], BF16)
            ysb = [sb(ph, "ysb%d" % i, [128, D]) for i in range(4)]
            def load_unit(gu):
                ex, u = gu // 4, gu % 4
                w = wgu[gu % 4]
                S.dma(sp, lambda e: e.dma_start(out=w[:, 0, :, :], in_=wg_s[ex, u]), [], [w], sembuf=w)
                S.dma(sp, lambda e: e.dma_start(out=w[:, 1, :, :], in_=wu_s[ex, u]), [], [w], sembuf=w)

            def load_wd(ex):
                w = wd[ex % 2]
                for hh in range(2):
                    S.dma(sp, lambda e, hh=hh: e.dma_start(out=w[:, hh * 8:(hh + 1) * 8, :],
                                                           in_=wd_s[ex][:, hh * 8:(hh + 1) * 8, :]),
                          [], [w], sembuf=w)

            def gather(ex):
                for c in range(4):
                    xt_ = xe[ex % 2][c]
                    S.dma(pool, lambda e, c=c, xt_=xt_: e.indirect_dma_start(
                        out=xt_[:], out_offset=None, in_=h2_d,
                        in_offset=bass.IndirectOffsetOnAxis(ap=idx_i[:, ex, c:c + 1], axis=0)),
                        [idx_i] + h2_b, [xt_], sembuf=xt_)
                    ag = affg[ex % 3][c]
                    S.dma(pool, lambda e, c=c, ag=ag: e.indirect_dma_start(
                        out=ag[:], out_offset=None, in_=aff_d,
                        in_offset=bass.IndirectOffsetOnAxis(ap=idx_i[:, ex, c:c + 1], axis=0)),
                        [idx_i] + affd_b, [ag], sembuf=ag)

            print("P4 sbuf left", nc.sbuf_bytes_remaining)
            gather(0)
            load_unit(0)
            load_unit(1)
            load_wd(0)
            load_unit(2)
            gather(1)
            yi = 0
            pending = []

            scat_b = [S.buf("scat%d" % i) for i in range(NE)]

            def flush_scatters():
                for (ex_, c_, y__) in pending:
                    rd = [y__, idx_i] + ([scat_b[ex_ - 1]] if ex_ > 0 else [])
                    S.dma(pool, lambda e, ex_=ex_, c_=c_, y__=y__: e.indirect_dma_start(
                        out=acc_d, out_offset=bass.IndirectOffsetOnAxis(ap=idx_i[:, ex_, c_:c_ + 1], axis=0),
                        in_=y__[:], in_offset=None, compute_op=ALU.add),
                        rd, [], sembuf=y__)
                    s_ = y__.sem
                    scat_b[ex_].w[id(s_)] = (s_, S.semval[s_])
                del pending[:]

            for ex in range(NE):
                for k in range(8):
                    bank = nps()
                    vw = bank[:].bitcast(BF16)
                    fns = [lambda e, c=c, vw=vw, k=k: e.transpose(vw[:, c * 128:(c + 1) * 128],
                                                                  xe[ex % 2][c][:, k * 128:(k + 1) * 128], ident_b[:])
                           for c in range(4)]
                    S.group(pe, fns, xe[ex % 2] + [ident_b], [bank])
                    if k % 2 == 0:
                        S.op(act, lambda e, vw=vw, k=k: e.activation(out=xeT[:, k, :], in_=vw[:, 0:512], func=AF.Copy),
                             [bank], [xeT])
                    else:
                        S.op(dve, lambda e, vw=vw, k=k: e.tensor_copy(out=xeT[:, k, :], in_=vw[:, 0:512]), [bank], [xeT])
                for u in range(4):
                    gu = ex * 4 + u
                    if gu + 3 < NE * 4:
                        load_unit(gu + 3)
                    if u == 1:
                        flush_scatters()
                    if u == 2 and ex + 2 < NE:
                        gather(ex + 2)
                    if u == 3 and ex + 1 < NE:
                        load_wd(ex + 1)
                    w = wgu[gu % 4]
                    for f in range(4):
                        fc = u * 4 + f
                        gb_ = nps()
                        mm_acc(gb_, gb_[:], [(w[:, 0, k, f * 128:(f + 1) * 128], xeT[:, k, :]) for k in range(8)], [w, xeT])
                        ub_ = nps()
                        mm_acc(ub_, ub_[:], [(w[:, 1, k, f * 128:(f + 1) * 128], xeT[:, k, :]) for k in range(8)], [w, xeT])
                        s_ = sg[fc % 2]
                        S.op(act, lambda e, gb_=gb_, s_=s_: e.activation(out=s_[:], in_=gb_[:], func=AF.Silu), [gb_], [s_])
                        S.op(dve, lambda e, ub_=ub_, s_=s_, fc=fc: e.tensor_tensor(out=hTm[:, fc, :], in0=ub_[:], in1=s_[:],
                                                                               op=ALU.mult), [ub_, s_], [hTm])
                wdt = wd[ex % 2]
                for c in range(4):
                    y_ = ysb[yi % 4]
                    yi += 1
                    ag = affg[ex % 3][c]
                    for h in range(2):
                        bank = nps()
                        sl = slice(h * 512, (h + 1) * 512)
                        mm_acc(bank, bank[:], [(hTm[:, fc, c * 128:(c + 1) * 128], wdt[:, fc, sl]) for fc in range(16)],
                               [hTm, wdt])
                        S.op(act, lambda e, bank=bank, sl=sl, y_=y_, ag=ag: e.activation(
                            out=y_[:, sl], in_=bank[:], func=AF.Copy, scale=ag[:, ex:ex + 1]), [bank, ag], [y_])
                        S.op(dve, lambda e, sl=sl, y_=y_: e.tensor_tensor(out=y_[:, sl], in0=y_[:, sl], in1=g2_bc[:, sl],
                                                                         op=ALU.mult), [y_, g2_bc], [y_])
                    pending.append((ex, c, y_))
            flush_scatters()
            S.end_phase()

        with ExitStack() as ph:
            G = 4
            fg = sb(ph, "fg", [128, D])
            xin = [sb(ph, "xin%d" % i, [128, D]) for i in range(2 * G)]
            xo = [sb(ph, "xo%d" % i, [128, D]) for i in range(2 * G)]
            junk = sb(ph, "junk", [128, D])
            sm = [sb(ph, "sm%d" % i, [128, 32]) for i in range(2)]
            load(sp, fg, fg[:], fg_d)
            for g in range(NT // G):
                st = sm[g % 2]
                S.op(dve, lambda e, st=st: e.memset(st[:, 0:G], 0.0), [], [st])
                for j in range(G):
                    t = g * G + j
                    xi = xin[t % (2 * G)]
                    load(sp, xi, xi[:], acc_d[t * 128:(t + 1) * 128, :], reads=[acc_b[t]])
                    S.op(act, lambda e, xi=xi, st=st, j=j: e.activation(out=junk[:], in_=xi[:], func=AF.Square,
                                                                        accum_out=st[:, j:j + 1]), [xi], [junk, st])
                rstd_from_ss(st, 0, G, n=G, sc=2 * G)
                for j in range(G):
                    t = g * G + j
                    xi = xin[t % (2 * G)]; xo_ = xo[t % (2 * G)]
                    S.op(dve, lambda e, xi=xi, xo_=xo_, st=st, j=j: e.scalar_tensor_tensor(
                        out=xo_[:], in0=xi[:], scalar=st[:, G + j:G + j + 1], in1=fg[:], op0=ALU.mult, op1=ALU.mult),
                        [xi, st, fg], [xo_])
                    S.dma(sp, lambda e, xo_=xo_, t=t: e.dma_start(out=y_d[t * 128:(t + 1) * 128, :], in_=xo_[:]),
                          [xo_], [], sembuf=xo_)
            S.end_phase()
    return nc


_NC_CACHE = {}


def _consts():
    c = {}
    tok = np.arange(128)[:, None] + 128 * np.arange(NT)[None, :]
    row = (tok // 64).astype(np.float64)
    col = (tok % 64).astype(np.float64)
    fr = 10000.0 ** (-np.arange(0, 32, 2, dtype=np.float64) / 32)
    ang = np.stack([row[..., None] * fr, col[..., None] * fr], axis=2)
    c["rope_cos"] = np.cos(ang).astype(np.float32)
    c["rope_sin"] = np.sin(ang).astype(np.float32)
    j = np.arange(128)[:, None]
    i = np.arange(128)[None, :]
    m0 = (j >= i).astype(np.float32)
    m1 = (j <= i).astype(np.float32)
    c["masks"] = np.stack([np.tile(m0, (1, 4)), np.tile(m1, (1, 4))], axis=1).astype(np.float32)
    c["ident"] = np.eye(128, dtype=np.float32)
    c["iota"] = np.tile(np.arange(128, dtype=np.float32)[None, :], (128, 1))
    c["pidx"] = np.arange(128, dtype=np.float32)[:, None].copy()
    c["tri"] = (j < i).astype(np.float32)
    return c


def _prep_inputs(inp):
    f = lambda a: np.ascontiguousarray(np.asarray(a, dtype=np.float32))
    bc = lambda v: np.ascontiguousarray(np.broadcast_to(np.asarray(v, np.float32)[None, :], (128, v.shape[-1])))
    fp = lambda v: np.ascontiguousarray(np.asarray(v, np.float32).reshape(-1, 128).T)
    shared = {}
    b_ada = np.asarray(inp["b_ada"], np.float32)[0]
    shared["w_ada"] = f(inp["w_ada"][0])
    shared["bada_fp"] = fp(b_ada[:2 * D])
    shared["bada_bc"] = bc(b_ada[2 * D:])
    shared["n1g_fp"] = fp(np.asarray(inp["norm1_g"])[0])
    shared["n2g_bc"] = bc(np.asarray(inp["norm2_g"])[0])
    shared["fg_bc"] = bc(np.asarray(inp["final_g"]))
    shared["lng_bc"] = bc(np.asarray(inp["gmlp_ln_g"])[0])
    shared["lnb_bc"] = bc(np.asarray(inp["gmlp_ln_b"])[0])
    shared["bmg_bc"] = bc(np.asarray(inp["b_merge_gate"])[0])
    ws = np.asarray(inp["w_spatial"], np.float32)[0]
    shared["wsT"] = np.ascontiguousarray(ws.transpose(2, 0, 1))
    shared["wsP"] = np.ascontiguousarray(ws.transpose(1, 0, 2))
    shared["bs_pg"] = np.ascontiguousarray(np.asarray(inp["b_spatial"], np.float32)[0].T)
    shared["sink_bc"] = bc(np.asarray(inp["attn_sink"])[0])
    wr = np.asarray(inp["w_router"], np.float32)[0]
    shared["wr"] = np.ascontiguousarray(wr.reshape(8, 128, NE).transpose(1, 0, 2))
    shared["w_in"] = f(inp["w_in"][0])
    shared["w_proj_a"] = f(inp["w_proj_a"][0])
    shared["w_proj_b"] = f(inp["w_proj_b"][0])
    shared["w_out"] = f(inp["w_out"][0])
    shared["w_exp_gate"] = f(inp["w_exp_gate"][0])
    shared["w_exp_up"] = f(inp["w_exp_up"][0])
    shared["w_exp_down"] = f(inp["w_exp_down"][0])
    shared.update(_consts())
    x = np.asarray(inp["x"], np.float32)
    c = np.asarray(inp["c"], np.float32)
    ctx = np.asarray(inp["ctx"], np.float32)
    c_ctx = np.asarray(inp["c_ctx"], np.float32)
    maps = []
    for b in range(x.shape[0]):
        m = dict(shared)
        m["x"] = np.ascontiguousarray(x[b])
        m["ctx"] = np.ascontiguousarray(ctx[b])
        cc = np.stack([c[b].reshape(8, 128).T, c_ctx.reshape(8, 128).T], axis=2)
        m["cc"] = np.ascontiguousarray(cc)
        maps.append(m)
    return maps


def kernel(**inputs):
    maps = _prep_inputs(inputs)
    if "nc" not in _NC_CACHE:
        _NC_CACHE["nc"] = _build(False)
    nc = _NC_CACHE["nc"]
    res = run_bass_kernel_spmd(nc, maps, core_ids=list(range(len(maps))))
    return np.stack([np.asarray(r["y"], dtype=np.float32) for r in res.results], axis=0)
```

```python
from contextlib import ExitStack
import numpy as np
import concourse.bass as bass
import concourse.mybir as mybir
from concourse.bass_utils import run_bass_kernel_spmd

F32 = mybir.dt.float32
BF16 = mybir.dt.bfloat16
I32 = mybir.dt.int32
AF = mybir.ActivationFunctionType
ALU = mybir.AluOpType
AX = mybir.AxisListType

D = 1024
L = 4096
NT = 32
CTX = 256
NE = 16
CAP = 512
DE = 2048
EPS = 1e-6
NBIS = 30


class Buf:
    __slots__ = ("name", "w", "r", "sem", "semval", "t")

    def __init__(self, name, t=None):
        self.name = name
        self.w = {}
        self.r = {}
        self.sem = None
        self.t = t

    def __getitem__(self, k):
        return self.t[k]


class Sched:
    COMPUTE = ("pe", "act", "dve", "pool")

    def __init__(self, nc, stack, nsem=84):
        self.nc = nc
        self.eng = {"pe": nc.tensor, "act": nc.scalar, "dve": nc.vector,
                    "pool": nc.gpsimd, "sp": nc.sync}
        self.esem = {}
        self.cnt = {}
        for e in self.COMPUTE:
            self.esem[e] = stack.enter_context(nc.semaphore("e_" + e))
            self.cnt[e] = 0
        self.free = [stack.enter_context(nc.semaphore("d%d" % i)) for i in range(nsem)]
        self.swfree = [self.free.pop() for _ in range(44)]
        self.swsems = set()
        self.semval = {}
        self.known = {e: {} for e in self.eng}
        self.allsems = {}
        self.phase_bufs = []

    def buf(self, name, t=None):
        b = Buf(name, t)
        self.phase_bufs.append(b)
        return b

    def _sem_of(self, b, sw=False):
        if b.sem is None:
            if sw:
                b.sem = self.swfree.pop()
                self.swsems.add(id(b.sem))
            else:
                b.sem = self.free.pop()
            self.semval.setdefault(b.sem, 0)
            self.allsems[id(b.sem)] = b.sem
        return b.sem

    def end_phase(self):
        targets = [(self.esem[e], self.cnt[e]) for e in self.COMPUTE if self.cnt[e] > 0]
        for s in self.allsems.values():
            if self.semval.get(s, 0) > 0:
                targets.append((s, self.semval[s]))
        for e, eng in self.eng.items():
            for s, v in targets:
                if s is self.esem.get(e):
                    continue
                if self.known[e].get(id(s), 0) < v:
                    eng.wait_ge(s, v)
                    self.known[e][id(s)] = v
        for b in self.phase_bufs:
            if b.sem is not None:
                if id(b.sem) not in self.swsems:
                    self.free.append(b.sem)
                b.sem = None
        self.phase_bufs = []

    def _collect(self, e, reads, writes):
        need = {}

        def add(d):
            for k, (s, v) in d.items():
                if k not in need or need[k][1] < v:
                    need[k] = (s, v)
        for b in reads:
            add(b.w)
        for b in writes:
            add(b.w)
            add(b.r)
        out = []
        for k, (s, v) in need.items():
            if e == "pe" and s is self.esem["pe"]:
                continue
            if self.known[e].get(k, 0) >= v:
                continue
            out.append((s, v))
            self.known[e][k] = v
        return out

    def _emit(self, e, fn, waits):
        eng = self.eng[e]
        for s, v in waits[1:]:
            eng.wait_ge(s, v)
        inst = fn(eng)
        if waits:
            inst._wait_ge(waits[0][0], waits[0][1])
        return inst

    def _record(self, ev, reads, writes):
        s, v = ev
        k = id(s)
        for b in reads:
            b.r[k] = (s, v)
        for b in writes:
            b.w = {k: (s, v)}
            b.r = {}

    def op(self, e, fn, reads=(), writes=()):
        waits = self._collect(e, reads, writes)
        inst = self._emit(e, fn, waits)
        self.cnt[e] += 1
        inst.then_inc(self.esem[e], 1)
        self._record((self.esem[e], self.cnt[e]), reads, writes)
        return inst

    def group(self, e, fns, reads=(), writes=()):
        waits = self._collect(e, reads, writes)
        eng = self.eng[e]
        for s, v in waits:
            eng.wait_ge(s, v)
        inst = None
        for fn in fns:
            inst = fn(eng)
        self.cnt[e] += 1
        inst.then_inc(self.esem[e], 1)
        self._record((self.esem[e], self.cnt[e]), reads, writes)

    def dma(self, q, fn, reads=(), writes=(), sembuf=None):
        waits = self._collect(q, reads, writes)
        inst = self._emit(q, fn, waits)
        s = self._sem_of(sembuf, sw=(q == "pool"))
        assert (id(s) in self.swsems) == (q == "pool"), sembuf.name
        self.semval[s] += 16
        inst.then_inc(s, 16)
        self._record((s, self.semval[s]), reads, writes)
        return inst


def _build(debug=False):
    nc = bass.Bass("TRN2", target_bir_lowering=False)
    KIN = "ExternalInput"

    def din(name, shape, dt=F32):
        return nc.dram_tensor(name, list(shape), dt, kind=KIN).ap()

    x_d = din("x", [L, D])
    ctx_d = din("ctx", [CTX, D])
    cc_d = din("cc", [128, 8, 2])
    wada_d = din("w_ada", [D, 6 * D])
    badafp_d = din("bada_fp", [128, 16])
    badabc_d = din("bada_bc", [128, 4 * D])
    n1g_d = din("n1g_fp", [128, 8])
    n2g_d = din("n2g_bc", [128, D])
    fg_d = din("fg_bc", [128, D])
    lng_d = din("lng_bc", [128, D])
    lnb_d = din("lnb_bc", [128, D])
    bmg_d = din("bmg_bc", [128, 2 * D])
    wsT_d = din("wsT", [128, 8, 128])
    wsP_d = din("wsP", [128, 8, 128])
    bs_d = din("bs_pg", [128, 8])
    sink_d = din("sink_bc", [128, 16])
    wr_d = din("wr", [128, 8, NE])
    win_d = din("w_in", [D, 5632])
    wpa_d = din("w_proj_a", [D, D])
    wpb_d = din("w_proj_b", [D, D])
    wout_d = din("w_out", [D, D])
    weg_d = din("w_exp_gate", [NE, D, DE])
    weu_d = din("w_exp_up", [NE, D, DE])
    wed_d = din("w_exp_down", [NE, DE, D])
    cos_d = din("rope_cos", [128, NT, 2, 16])
    sin_d = din("rope_sin", [128, NT, 2, 16])
    mask_d = din("masks", [128, 2, 512])
    ident_d = din("ident", [128, 128])
    iota_d = din("iota", [128, 128])
    pidx_d = din("pidx", [128, 1])
    tri_d = din("tri", [128, 128])
    y_d = nc.dram_tensor("y", [L, D], F32, kind="ExternalOutput").ap()
    skind = "ExternalOutput" if debug else "Internal"
    ma_d = nc.dram_tensor("ma_s", [L, D], F32, kind=skind).ap()
    acc_d = nc.dram_tensor("acc_s", [L, D], F32, kind=skind).ap()
    h2_d = nc.dram_tensor("h2_s", [L, D], BF16, kind=skind).ap()
    aff_d = nc.dram_tensor("aff_s", [L, NE], F32, kind=skind).ap()
    wg_s = nc.dram_tensor("wg_s", [NE, 4, 128, 8, 512], BF16, kind="Internal").ap()
    wu_s = nc.dram_tensor("wu_s", [NE, 4, 128, 8, 512], BF16, kind="Internal").ap()
    wd_s = nc.dram_tensor("wd_s", [NE, 128, 16, D], BF16, kind="Internal").ap()
    g1_s = nc.dram_tensor("g1_s", [128, D], F32, kind="Internal").ap()
    if debug:
        dbg_d = nc.dram_tensor("dbg", [128, 4096], F32, kind="ExternalOutput").ap()

    top = ExitStack()
    with top:
        S = Sched(nc, top)
        pe, act, dve, pool, sp = "pe", "act", "dve", "pool", "sp"

        uniq = [0]

        def sb(stack, name, shape, dt=F32):
            uniq[0] += 1
            t = stack.enter_context(nc.sbuf_tensor("s%d_%s" % (uniq[0], name), list(shape), dt))
            return S.buf(name, t)

        def psbanks(stack):
            return [S.buf("ps%d" % i, stack.enter_context(nc.psum_tensor("ps%d" % i, [128, 512], F32)))
                    for i in range(8)]

        ident_f = sb(top, "ident_f", [128, 128])
        ident_b = sb(top, "ident_b", [128, 128], BF16)
        A1 = sb(top, "A1", [128, 8, 2])
        B1 = sb(top, "B1", [128, 8, 2])
        A2_bc = sb(top, "A2_bc", [128, D])
        B2_bc = sb(top, "B2_bc", [128, D])
        g2_bc = sb(top, "g2_bc", [128, D])
        aff_all = sb(top, "aff_all", [128, NT, NE])
        idx_i = sb(top, "idx_i", [128, NE, 4], I32)
        ones_f = sb(top, "ones_f", [128, 128])
        ones_b = sb(top, "ones_b", [128, 128], BF16)
        epsb = sb(top, "epsb", [128, 1])
        ps = psbanks(top)
        psi = [0]

        psel = [None]
        pcnt = {}

        def nps():
            pool_ = psel[0]
            if pool_ is None:
                b = ps[psi[0] % 8]
                psi[0] += 1
                return b
            k = pcnt.get(pool_, 0)
            pcnt[pool_] = k + 1
            return ps[pool_[k % len(pool_)]]

        ma_b = [S.buf("ma%d" % t) for t in range(NT)]
        acc_b = [S.buf("acc%d" % t) for t in range(NT)]
        h2_b = [S.buf("h2d%d" % t) for t in range(NT)]
        affd_b = [S.buf("affd%d" % t) for t in range(NT)]
        dbg_b = S.buf("dbg")
        g1d_b = S.buf("g1d")
        S.phase_bufs = []

        def load(q, dst, dst_ap, src_ap, reads=()):
            S.dma(q, lambda e: e.dma_start(out=dst_ap, in_=src_ap), reads=reads, writes=[dst], sembuf=dst)

        load(sp, ident_f, ident_f[:], ident_d)
        S.op(dve, lambda e: e.tensor_copy(out=ident_b[:], in_=ident_f[:]), [ident_f], [ident_b])
        S.op(dve, lambda e: e.memset(ones_f[:], 1.0), [], [ones_f])
        S.op(dve, lambda e: e.memset(ones_b[:], 1.0), [], [ones_b])
        S.op(dve, lambda e: e.memset(epsb[:], EPS), [], [epsb])

        def rstd_from_ss(st, ci, co, n=1, sc=8):
            v = st[:, sc:sc + n]; ti = st[:, sc + n:sc + 2 * n].bitcast(I32); tt = st[:, sc + 2 * n:sc + 3 * n]
            y = st[:, co:co + n]
            S.op(dve, lambda e: e.tensor_scalar(out=v, in0=st[:, ci:ci + n], scalar1=1.0 / D, scalar2=EPS,
                                                op0=ALU.mult, op1=ALU.add), [st], [st])
            S.op(dve, lambda e: e.tensor_single_scalar(out=ti, in_=v.bitcast(I32), scalar=1,
                                                       op=ALU.arith_shift_right), [st], [st])
            S.op(dve, lambda e: e.tensor_scalar(out=y.bitcast(I32), in0=ti, scalar1=-1.0,
                                                scalar2=float(0x5f3759df), op0=ALU.mult, op1=ALU.add), [st], [st])
            for _ in range(3):
                S.op(dve, lambda e: e.tensor_tensor(out=tt, in0=y, in1=y, op=ALU.mult), [st], [st])
                S.op(dve, lambda e: e.tensor_tensor(out=tt, in0=tt, in1=v, op=ALU.mult), [st], [st])
                S.op(dve, lambda e: e.tensor_scalar(out=tt, in0=tt, scalar1=-0.5, scalar2=1.5, op0=ALU.mult,
                                                    op1=ALU.add), [st], [st])
                S.op(dve, lambda e: e.tensor_tensor(out=y, in0=y, in1=tt, op=ALU.mult), [st], [st])

        def sumsq(xt, junk, st, ci):
            S.op(dve, lambda e: e.memset(st[:, ci:ci + 1], 0.0), [], [st])
            S.op(act, lambda e: e.activation(out=junk[:], in_=xt[:], func=AF.Square, accum_out=st[:, ci:ci + 1]),
                 [xt], [junk, st])

        def transpose_evac(src, col0, ncols, dtype, evac):
            nblk = ncols // 128
            for k0 in range(0, nblk, 4):
                n = min(4, nblk - k0)
                bank = nps()
                idt = ident_f if dtype == F32 else ident_b
                if dtype == F32:
                    view = bank[:, 0:n * 128]
                else:
                    view = bank[:].bitcast(BF16)[:, 0:n * 128]
                fns = []
                for j in range(n):
                    c = col0 + (k0 + j) * 128
                    fns.append(lambda e, j=j, c=c: e.transpose(view[:, j * 128:(j + 1) * 128],
                                                              src[:, c:c + 128], idt[:]))
                S.group(pe, fns, [src, idt], [bank])
                evac(bank, view, k0, n)
                yield

        def drain(gen):
            for _ in gen:
                pass

        def interleave(gens, pools=None):
            items = [(g, (pools[i] if pools else None)) for i, g in enumerate(gens) if g is not None]
            while items:
                for it in list(items):
                    psel[0] = it[1]
                    try:
                        next(it[0])
                    except StopIteration:
                        items.remove(it)
            psel[0] = None

        def norm1_pro(xt, st, xhat):
            sumsq(xt, xhat, st, 0)
            rstd_from_ss(st, 0, 1)
            S.op(dve, lambda e: e.tensor_scalar(out=xhat[:], in0=xt[:], scalar1=st[:, 1:2], scalar2=None,
                                                op0=ALU.mult), [xt, st], [xhat])

        def norm1_T(xhat, hT, col):
            def evac(bank, view, k0, n):
                for j in range(n):
                    k = k0 + j
                    if k0 == 0:
                        S.op(act, lambda e, j=j, k=k: e.activation(
                            out=hT[:, k, :], in_=view[:, j * 128:(j + 1) * 128], func=AF.Identity,
                            bias=B1[:, k, col:col + 1], scale=A1[:, k, col:col + 1]), [bank, A1, B1], [hT])
                    else:
                        S.op(dve, lambda e, j=j, k=k: e.tensor_scalar(
                            out=hT[:, k, :], in0=view[:, j * 128:(j + 1) * 128], scalar1=A1[:, k, col:col + 1],
                            scalar2=B1[:, k, col:col + 1], op0=ALU.mult, op1=ALU.add), [bank, A1, B1], [hT])
            yield from transpose_evac(xhat, 0, D, F32, evac)

        def norm1_hT(xt, junk, st, xhat, hT, col):
            norm1_pro(xt, st, xhat)
            yield from norm1_T(xhat, hT, col)

        def mm_acc(out_bank, out_ap, pairs, reads):
            n = len(pairs)
            fns = [lambda e, i=i, l=l, r=r: e.matmul(out_ap, l, r, start=(i == 0), stop=(i == n - 1))
                   for i, (l, r) in enumerate(pairs)]
            S.group(pe, fns, reads, [out_bank])

        winv = win_d.rearrange("(k p) f -> p k f", p=128)

        def load_w(dst, dst_ap, src_view, c0, c1):
            for k in range(8):
                S.dma(pool, lambda e, k=k: e.dma_start(out=dst_ap[:, k, :], in_=src_view[:, k, c0:c1]),
                      [], [dst], sembuf=dst)

        wgv = weg_d.rearrange("e (k p) f -> e p k f", p=128)
        wuv = weu_d.rearrange("e (k p) f -> e p k f", p=128)
        wdv = wed_d.rearrange("e (c p) f -> e p c f", p=128)
        castb = S.buf("castb")
        cast_jobs = []
        for ex_ in range(NE):
            for u_ in range(4):
                cast_jobs.append((wg_s[ex_, u_], wgv[ex_, :, :, u_ * 512:(u_ + 1) * 512]))
                cast_jobs.append((wu_s[ex_, u_], wuv[ex_, :, :, u_ * 512:(u_ + 1) * 512]))
            for hh_ in range(2):
                cast_jobs.append((wd_s[ex_][:, hh_ * 8:(hh_ + 1) * 8, :], wdv[ex_, :, hh_ * 8:(hh_ + 1) * 8, :]))
        cast_pos = [0]

        def cast_some(upto):
            upto = min(upto, len(cast_jobs))
            while cast_pos[0] < upto:
                o_, i_ = cast_jobs[cast_pos[0]]
                S.dma(pool, lambda e, o_=o_, i_=i_: e.dma_start(out=o_, in_=i_), [], [], sembuf=castb)
                cast_pos[0] += 1

        p1w = ExitStack()
        w1 = sb(p1w, "w1", [128, 8, 3072], BF16)
        wpa = sb(p1w, "wpa", [128, 8, D], BF16)
        wsT = sb(p1w, "wsT", [128, 8, 128], BF16)
        load_w(w1, w1[:, :, 0:2048], winv, 0, 2048)
        load_w(w1, w1[:, :, 2048:3072], winv, 3584, 4608)
        load_w(wpa, wpa[:], wpa_d.rearrange("(k p) f -> p k f", p=128), 0, D)
        S.dma(pool, lambda e: e.dma_start(out=wsT[:], in_=wsT_d), [], [wsT], sembuf=wsT)
        S.phase_bufs = []

        with ExitStack() as ph:
            cc = sb(ph, "cc", [128, 8, 2])
            sc = sb(ph, "sc", [128, 8, 2])
            sc_rep = sb(ph, "sc_rep", [128, 8, 128])
            bada_fp = sb(ph, "bada_fp", [128, 16])
            bada_bc = sb(ph, "bada_bc", [128, 4 * D])
            n1g = sb(ph, "n1g", [128, 8])
            n2g = sb(ph, "n2g", [128, D])
            modfp = sb(ph, "modfp", [128, 16, 2])
            modbc = sb(ph, "modbc", [128, 4 * D])
            wblk = [sb(ph, "wblk%d" % i, [128, 8, 512]) for i in range(2)]
            load(sp, cc, cc[:], cc_d)
            load(sp, bada_fp, bada_fp[:], badafp_d)
            load(sp, bada_bc, bada_bc[:], badabc_d)
            load(sp, n1g, n1g[:], n1g_d)
            load(sp, n2g, n2g[:], n2g_d)
            S.op(act, lambda e: e.activation(out=sc[:], in_=cc[:], func=AF.Silu), [cc], [sc])
            for k in range(8):
                S.op(dve, lambda e, k=k: e.tensor_copy(out=sc_rep[:, k, :],
                                                       in_=sc[:, k, 0:1].to_broadcast([128, 128])),
                     [sc], [sc_rep])
            wav = wada_d.rearrange("(k p) f -> p k f", p=128)
            for blk in range(12):
                wb = wblk[blk % 2]
                load(sp, wb, wb[:], wav[:, :, blk * 512:(blk + 1) * 512])
                if blk < 4:
                    for j in range(4):
                        cj = blk * 4 + j
                        bank = nps()
                        mm_acc(bank, bank[:, 0:2],
                               [(wb[:, k, j * 128:(j + 1) * 128], sc[:, k, :]) for k in range(8)], [wb, sc])
                        S.op(dve, lambda e, cj=cj, bank=bank: e.tensor_scalar(
                            out=modfp[:, cj, :], in0=bank[:, 0:2], scalar1=bada_fp[:, cj:cj + 1], scalar2=None,
                            op0=ALU.add), [bank, bada_fp], [modfp])
                else:
                    c0 = (blk - 4) * 512
                    bank = nps()
                    mm_acc(bank, bank[:], [(sc_rep[:, k, :], wb[:, k, :]) for k in range(8)], [wb, sc_rep])
                    S.op(dve, lambda e, c0=c0, bank=bank: e.tensor_tensor(
                        out=modbc[:, c0:c0 + 512], in0=bank[:], in1=bada_bc[:, c0:c0 + 512], op=ALU.add),
                        [bank, bada_bc], [modbc])
            S.op(dve, lambda e: e.tensor_scalar(out=A1[:], in0=modfp[:, 8:16, :], scalar1=1.0, scalar2=None,
                                                op0=ALU.add), [modfp], [A1])
            S.op(dve, lambda e: e.tensor_tensor(out=A1[:], in0=A1[:],
                                                in1=n1g[:].unsqueeze(2).to_broadcast([128, 8, 2]), op=ALU.mult),
                 [A1, n1g], [A1])
            S.op(dve, lambda e: e.tensor_copy(out=B1[:], in_=modfp[:, 0:8, :]), [modfp], [B1])
            S.dma(sp, lambda e: e.dma_start(out=g1_s, in_=modbc[:, 0:D]), [modbc], [g1d_b], sembuf=modbc)
            S.op(dve, lambda e: e.tensor_copy(out=B2_bc[:], in_=modbc[:, D:2 * D]), [modbc], [B2_bc])
            S.op(dve, lambda e: e.tensor_scalar(out=A2_bc[:], in0=modbc[:, 2 * D:3 * D], scalar1=1.0, scalar2=None,
                                                op0=ALU.add), [modbc], [A2_bc])
            S.op(dve, lambda e: e.tensor_tensor(out=A2_bc[:], in0=A2_bc[:], in1=n2g[:], op=ALU.mult),
                 [A2_bc, n2g], [A2_bc])
            S.op(dve, lambda e: e.tensor_copy(out=g2_bc[:], in_=modbc[:, 3 * D:4 * D]), [modbc], [g2_bc])
            if debug:
                S.dma(sp, lambda e: e.dma_start(out=dbg_d[:, 0:16], in_=A1[:].rearrange("p a b -> p (a b)")),
                      [A1], [dbg_b], sembuf=A1)
                S.dma(sp, lambda e: e.dma_start(out=dbg_d[:, 16:32], in_=B1[:].rearrange("p a b -> p (a b)")),
                      [B1], [dbg_b], sembuf=B1)
                S.dma(sp, lambda e: e.dma_start(out=dbg_d[:, 1024:2048], in_=A2_bc[:]), [A2_bc], [dbg_b], sembuf=A2_bc)
                S.dma(sp, lambda e: e.dma_start(out=dbg_d[:, 2048:3072], in_=g2_bc[:]), [g2_bc], [dbg_b], sembuf=g2_bc)
            print("P0 sbuf left", nc.sbuf_bytes_remaining)
            S.end_phase()

        with ExitStack() as ph:
            wsP = sb(ph, "wsP", [128, 8, 128])
            rs = sb(ph, "rs", [128, 8])
            bs = sb(ph, "bs", [128, 8])
            lng = sb(ph, "lng", [128, D])
            lnb = sb(ph, "lnb", [128, D])
            biasA = sb(ph, "biasA", [128, D])
            bga = sb(ph, "bga", [128, D])
            xts = [sb(ph, "xt%d" % i, [128, D]) for i in range(3)]
            junk = sb(ph, "junk", [128, D])
            xhats = [sb(ph, "xhat%d" % i, [128, D]) for i in range(2)]
            hTs = [sb(ph, "hT%d" % i, [128, 8, 128], BF16) for i in range(2)]
            us = [sb(ph, "u%d" % i, [128, D]) for i in range(2)]
            vs = sb(ph, "v", [128, D])
            vhs = [sb(ph, "vh%d" % i, [128, D], BF16) for i in range(2)]
            gas = [sb(ph, "ga%d" % i, [128, D]) for i in range(3)]
            t1 = sb(ph, "t1", [128, D])
            yas = [sb(ph, "ya%d" % i, [128, D], BF16) for i in range(2)]
            yaT = sb(ph, "yaT", [128, 8, 128], BF16)
            mas = [sb(ph, "mas%d" % i, [128, D]) for i in range(2)]
            sm = [sb(ph, "sm%d" % i, [128, 16]) for i in range(3)]

            load(sp, wsP, wsP[:], wsP_d)
            load(sp, bs, bs[:], bs_d)
            load(sp, lng, lng[:], lng_d)
            load(sp, lnb, lnb[:], lnb_d)
            load(sp, bga, bga[:], bmg_d[:, 0:D])
            bga_row = sb(ph, "bga_row", [1, D], BF16)
            S.dma(pool, lambda e: e.dma_start(out=bga_row[:], in_=bmg_d[0:1, 0:D]), [], [bga_row], sembuf=bga_row)
            S.op(dve, lambda e: e.tensor_reduce(out=rs[:], in_=wsP[:], axis=AX.X, op=ALU.add), [wsP], [rs])
            for g in range(8):
                S.op(dve, lambda e, g=g: e.tensor_scalar(
                    out=biasA[:, g * 128:(g + 1) * 128], in0=lnb[:, g * 128:(g + 1) * 128],
                    scalar1=rs[:, g:g + 1], scalar2=bs[:, g:g + 1], op0=ALU.mult, op1=ALU.add),
                    [lnb, rs, bs], [biasA])

            def p1_ld(t):
                xt = xts[t % 3]
                load(sp, xt, xt[:], x_d[t * 128:(t + 1) * 128, :])

            def p1_pro(t):
                if t + 1 < NT:
                    p1_ld(t + 1)
                norm1_pro(xts[t % 3], sm[t % 3], xhats[t % 2])

            def p1_A(t):
                hT = hTs[t % 2]; st = sm[t % 3]
                u = us[t % 2]; vh = vhs[t % 2]; ga = gas[t % 3]
                yield from norm1_T(xhats[t % 2], hT, 0)
                if t + 1 < NT:
                    p1_pro(t + 1)
                yield
                S.op(dve, lambda e: e.memset(st[:, 2:4], 0.0), [], [st])

                def grp(gi):
                    bank = nps()
                    h = gi % 2
                    pairs = [(hT[:, k, :], w1[:, k, gi * 512:(gi + 1) * 512]) for k in range(8)]
                    if gi >= 4:
                        pairs.append((ones_b[0:1, :], bga_row[0:1, h * 512:(h + 1) * 512]))
                    mm_acc(bank, bank[:], pairs, [hT, w1, ones_b, bga_row])
                    if gi < 2:
                        S.op(act, lambda e, bank=bank, h=h: e.activation(
                            out=u[:, h * 512:(h + 1) * 512], in_=bank[:], func=AF.Gelu_apprx_tanh), [bank], [u])
                    elif gi < 4:
                        S.op(act, lambda e, bank=bank, h=h: e.activation(
                            out=vs[:, h * 512:(h + 1) * 512], in_=bank[:], func=AF.Gelu_apprx_tanh,
                            accum_out=st[:, 2 + h:3 + h]), [bank], [vs, st])
                    else:
                        S.op(act, lambda e, bank=bank, h=h: e.activation(
                            out=ga[:, h * 512:(h + 1) * 512], in_=bank[:], func=AF.Tanh, scale=0.5), [bank], [ga])

                grp(2)
                yield
                grp(3)
                S.op(dve, lambda e: e.tensor_tensor(out=st[:, 4:5], in0=st[:, 2:3], in1=st[:, 3:4], op=ALU.add),
                     [st], [st])
                S.op(dve, lambda e: e.tensor_scalar(out=st[:, 4:5], in0=st[:, 4:5], scalar1=-1.0 / D, scalar2=None,
                                                    op0=ALU.mult), [st], [st])
                S.op(dve, lambda e: e.memset(st[:, 5:6], 0.0), [], [st])
                S.op(act, lambda e: e.activation(out=junk[:], in_=vs[:], func=AF.Square, bias=st[:, 4:5],
                                                 accum_out=st[:, 5:6]), [vs, st], [junk, st])
                rstd_from_ss(st, 5, 6)
                S.op(dve, lambda e: e.tensor_scalar(out=vh[:], in0=vs[:], scalar1=st[:, 4:5], scalar2=st[:, 6:7],
                                                    op0=ALU.add, op1=ALU.mult), [vs, st], [vh])
                yield
                for gi in (0, 1, 4, 5):
                    grp(gi)
                    yield

            def p1_B(t):
                u = us[t % 2]; vh = vhs[t % 2]; ya = yas[t % 2]
                for h in range(2):
                    bank = nps()
                    fns = [lambda e, g=g, bank=bank: e.matmul(
                        bank[:, (g % 4) * 128:(g % 4 + 1) * 128], wsT[:, g, :], vh[:, g * 128:(g + 1) * 128],
                        start=True, stop=True) for g in range(h * 4, h * 4 + 4)]
                    S.group(pe, fns, [wsT, vh], [bank])
                    sl = slice(h * 512, (h + 1) * 512)
                    S.op(dve, lambda e, bank=bank, sl=sl: e.tensor_tensor(out=t1[:, sl], in0=bank[:], in1=lng[:, sl],
                                                                         op=ALU.mult), [bank, lng], [t1])
                    S.op(dve, lambda e, sl=sl: e.tensor_tensor(out=t1[:, sl], in0=t1[:, sl], in1=biasA[:, sl],
                                                               op=ALU.add), [t1, biasA], [t1])
                    S.op(dve, lambda e, sl=sl: e.tensor_tensor(out=ya[:, sl], in0=t1[:, sl], in1=u[:, sl],
                                                              op=ALU.mult), [t1, u], [ya])
                    yield
                    yield

            def p1_C(t):
                ga = gas[t % 3]; ma = mas[t % 2]; ya = yas[t % 2]

                def evac(bank, view, k0, n):
                    S.op(act, lambda e: e.activation(out=yaT[:, k0:k0 + n, :].rearrange("p a b -> p (a b)"),
                                                     in_=view, func=AF.Copy), [bank], [yaT])
                yield from transpose_evac(ya, 0, D, BF16, evac)
                yield
                for h in range(2):
                    bank = nps()
                    sl = slice(h * 512, (h + 1) * 512)
                    mm_acc(bank, bank[:], [(yaT[:, k, :], wpa[:, k, sl]) for k in range(8)], [yaT, wpa])
                    S.op(dve, lambda e, bank=bank, sl=sl: e.scalar_tensor_tensor(
                        out=ma[:, sl], in0=ga[:, sl], scalar=1.0, in1=bank[:], op0=ALU.add, op1=ALU.mult),
                        [bank, ga], [ma])
                    yield
                S.dma(sp, lambda e: e.dma_start(out=ma_d[t * 128:(t + 1) * 128, :], in_=ma[:]),
                      [ma], [ma_b[t]], sembuf=ma)

            print("P1 sbuf left", nc.sbuf_bytes_remaining)
            p1_ld(0)
            p1_pro(0)
            for t in range(NT + 2):
                cast_some(int(len(cast_jobs) * (t + 1) / 66.0))
                interleave([p1_A(t) if t < NT else None, p1_B(t - 1) if 0 <= t - 1 < NT else None,
                            p1_C(t - 2) if 0 <= t - 2 < NT else None],
                           pools=[(0, 1, 2, 3), (4, 5), (6, 7)])
            S.end_phase()
        p1w.close()

        with ExitStack() as ph:
            w2 = sb(ph, "w2", [128, 8, 2560], BF16)
            wpb = sb(ph, "wpb", [128, 8, D], BF16)
            wo = sb(ph, "wo", [128, 8, D], BF16)
            cosT = sb(ph, "cosT", [128, NT, 2, 16])
            sinT = sb(ph, "sinT", [128, NT, 2, 16])
            mask = sb(ph, "mask", [128, 2, 512], BF16)
            bgb = sb(ph, "bgb", [128, D])
            esink = sb(ph, "esink", [128, 16])
            wr = sb(ph, "wr", [128, 8, NE])
            xts = [sb(ph, "xt%d" % i, [128, D]) for i in range(2)]
            xres = sb(ph, "xres", [128, D])
            xhats = [sb(ph, "xhat%d" % i, [128, D]) for i in range(2)]
            hTs = [sb(ph, "hT%d" % i, [128, 8, 128], BF16) for i in range(4)]
            qsb = sb(ph, "qsb", [128, 1280])
            rt = [sb(ph, "rt%d" % i, [128, 256]) for i in range(4)]
            qrot = sb(ph, "qrot", [128, 1280], BF16)
            QTs = [sb(ph, "QT%d" % i, [64, 16, 128], BF16) for i in range(3)]
            KTs = [sb(ph, "KT%d" % i, [64, 4, 128], BF16) for i in range(4)]
            KTc = [sb(ph, "KTc%d" % i, [64, 4, 128], BF16) for i in range(2)]
            Vs = [sb(ph, "V%d" % i, [128, 4, 65], BF16) for i in range(4)]
            Vc = [sb(ph, "Vc%d" % i, [128, 4, 65], BF16) for i in range(2)]
            gb = sb(ph, "gb", [128, D])
            PTs = [sb(ph, "PT%d" % i, [128, 512], BF16) for i in range(10)]
            den = sb(ph, "den", [128, 8])
            yb = sb(ph, "yb", [128, D], BF16)
            ybTs = [sb(ph, "ybT%d" % i, [128, 8, 128], BF16) for i in range(2)]
            mat = sb(ph, "mat0", [128, D])
            mgb = sb(ph, "mgb", [128, D], BF16)
            mgT = sb(ph, "mgT", [128, 8, 128], BF16)
            h2fs = [sb(ph, "h2f%d" % i, [128, D]) for i in range(2)]
            g1_bc = h2fs[1]
            load(sp, g1_bc, g1_bc[:], g1_s, reads=[g1d_b])
            wtmp = xts
            h2b = sb(ph, "h2b", [128, D], BF16)
            h2T = sb(ph, "h2T", [128, 8, 128])
            smA = [sb(ph, "smA%d" % i, [128, 16]) for i in range(3)]
            smBs = [sb(ph, "smB%d" % i, [128, 16]) for i in range(2)]
            lg = sb(ph, "lg", [128, NE])
            lg2 = sb(ph, "lg2", [128, NE])

            load_w(w2, w2[:, :, 0:1536], winv, 2048, 3584)
            load_w(w2, w2[:, :, 1536:2560], winv, 4608, 5632)
            load_w(wpb, wpb[:], wpb_d.rearrange("(k p) f -> p k f", p=128), 0, D)
            wov = wout_d.rearrange("(k p) f -> p k f", p=128)
            for k in range(8):
                wt_ = wtmp[k % 2]
                load(sp, wt_, wt_[:], wov[:, k, :])
                S.op(dve, lambda e, k=k, wt_=wt_: e.scalar_tensor_tensor(
                    out=wo[:, k, :], in0=wt_[:], scalar=0.5, in1=g1_bc[:], op0=ALU.mult, op1=ALU.mult),
                    [wt_, g1_bc], [wo])
            load(sp, cosT, cosT[:], cos_d)
            load(sp, sinT, sinT[:], sin_d)
            S.dma(pool, lambda e: e.dma_start(out=mask[:], in_=mask_d), [], [mask], sembuf=mask)
            load(sp, bgb, bgb[:], bmg_d[:, D:2 * D])
            bgb_row = sb(ph, "bgb_row", [1, D], BF16)
            S.dma(pool, lambda e: e.dma_start(out=bgb_row[:], in_=bmg_d[0:1, D:2 * D]), [], [bgb_row], sembuf=bgb_row)
            load(sp, esink, esink[:], sink_d)
            S.op(act, lambda e: e.activation(out=esink[:], in_=esink[:], func=AF.Exp), [esink], [esink])
            load(sp, wr, wr[:], wr_d)
            for v_ in Vs + Vc:
                S.op(dve, lambda e, v_=v_: e.memset(v_[:], 1.0), [], [v_])

            def rope(src, c0, nh, t, dst):
                sv = src[:, c0:c0 + nh * 64].rearrange("p (h a f i) -> p h a f i", h=nh, a=2, f=2, i=16)
                dv = dst[:, c0:c0 + nh * 64].rearrange("p (h a f i) -> p h a f i", h=nh, a=2, f=2, i=16)
                x1 = sv[:, :, :, 0, :]
                x2 = sv[:, :, :, 1, :]
                cb = cosT[:, t].unsqueeze(1).to_broadcast([128, nh, 2, 16])
                sn = sinT[:, t].unsqueeze(1).to_broadcast([128, nh, 2, 16])
                n = nh * 32
                tv = [r_[:, 0:n].rearrange("p (h a i) -> p h a i", h=nh, a=2, i=16) for r_ in rt]
                S.op(dve, lambda e: e.tensor_tensor(out=tv[0], in0=x1, in1=cb, op=ALU.mult), [src, cosT], [rt[0]])
                S.op(dve, lambda e: e.tensor_tensor(out=tv[1], in0=x2, in1=sn, op=ALU.mult), [src, sinT], [rt[1]])
                S.op(dve, lambda e: e.tensor_tensor(out=dv[:, :, :, 0, :], in0=tv[0], in1=tv[1], op=ALU.subtract),
                     [rt[0], rt[1]], [dst])
                S.op(dve, lambda e: e.tensor_tensor(out=tv[2], in0=x1, in1=sn, op=ALU.mult), [src, sinT], [rt[2]])
                S.op(dve, lambda e: e.tensor_tensor(out=tv[3], in0=x2, in1=cb, op=ALU.mult), [src, cosT], [rt[3]])
                S.op(dve, lambda e: e.tensor_tensor(out=dv[:, :, :, 1, :], in0=tv[2], in1=tv[3], op=ALU.add),
                     [rt[2], rt[3]], [dst])

            def kv_part(hT_, t, KT, V, do_rope):
                bank = nps()
                mm_acc(bank, bank[:], [(hT_[:, k, :], w2[:, k, 1024:1536]) for k in range(8)], [hT_, w2])
                S.op(act, lambda e: e.activation(out=qsb[:, 1024:1280], in_=bank[:, 0:256], func=AF.Copy),
                     [bank], [qsb])
                S.op(act, lambda e: e.activation(out=V[:, :, 0:64],
                                                 in_=bank[:, 256:512].rearrange("p (h d) -> p h d", h=4),
                                                 func=AF.Copy), [bank], [V])
                if do_rope:
                    rope(qsb, 1024, 4, t, qrot)
                else:
                    S.op(dve, lambda e: e.tensor_copy(out=qrot[:, 1024:1280], in_=qsb[:, 1024:1280]), [qsb], [qrot])
                yield
                yield
                bank2 = nps()
                vw = bank2[:].bitcast(BF16)
                fns = [lambda e, h=h: e.transpose(vw[0:64, h * 128:(h + 1) * 128],
                                                  qrot[:, 1024 + h * 64:1024 + (h + 1) * 64], ident_b[:])
                       for h in range(4)]
                S.group(pe, fns, [qrot, ident_b], [bank2])
                S.op(act, lambda e: e.activation(out=KT[:].rearrange("p a b -> p (a b)"), in_=vw[0:64, 0:512],
                                                 func=AF.Copy), [bank2], [KT])
                yield

            for ci in range(2):
                xt = xts[ci]
                load(sp, xt, xt[:], ctx_d[ci * 128:(ci + 1) * 128, :])
                drain(norm1_hT(xt, None, smA[ci], xhats[0], hTs[0], 1))
                drain(kv_part(hTs[0], 0, KTc[ci], Vc[ci], False))

            def p2_pro(t):
                xt = xts[t % 2]
                load(sp, xt, xt[:], x_d[t * 128:(t + 1) * 128, :])
                norm1_pro(xt, smA[t % 3], xhats[t % 2])

            def p2_A(t):
                QT = QTs[t % 3]; hT = hTs[t % 4]
                yield from norm1_T(xhats[t % 2], hT, 0)
                if t + 1 < NT:
                    p2_pro(t + 1)
                yield
                for h in range(2):
                    bank = nps()
                    mm_acc(bank, bank[:], [(hT[:, k, :], w2[:, k, h * 512:(h + 1) * 512]) for k in range(8)], [hT, w2])
                    S.op(act, lambda e, bank=bank, h=h: e.activation(out=qsb[:, h * 512:(h + 1) * 512], in_=bank[:],
                                                                     func=AF.Copy), [bank], [qsb])
                    yield
                rope(qsb, 0, 8, t, qrot)
                rope(qsb, 512, 8, t, qrot)
                yield
                yield from kv_part(hT, t, KTs[t % 4], Vs[t % 4], True)
                yield
                yield
                yield
                for h4 in range(4):
                    bank = nps()
                    vw = bank[:].bitcast(BF16)
                    fns = [lambda e, j=j, vw=vw: e.transpose(vw[0:64, j * 128:(j + 1) * 128],
                                                             qrot[:, (h4 * 4 + j) * 64:(h4 * 4 + j + 1) * 64], ident_b[:])
                           for j in range(4)]
                    S.group(pe, fns, [qrot, ident_b], [bank])
                    if h4 % 2 == 0:
                        S.op(act, lambda e, vw=vw: e.activation(
                            out=QT[:, h4 * 4:h4 * 4 + 4, :].rearrange("p a b -> p (a b)"), in_=vw[0:64, 0:512],
                            func=AF.Copy), [bank], [QT])
                    else:
                        S.op(dve, lambda e, vw=vw: e.tensor_copy(
                            out=QT[:, h4 * 4:h4 * 4 + 4, :].rearrange("p a b -> p (a b)"), in_=vw[0:64, 0:512]),
                            [bank], [QT])
                    yield

            def p2_B1(t):
                QT = QTs[t % 3]; ybT = ybTs[t % 2]
                keys = []
                if t > 0:
                    keys.append((KTs[(t - 1) % 4], Vs[(t - 1) % 4], 0))
                keys.append((KTs[t % 4], Vs[t % 4], None))
                if t < NT - 1:
                    keys.append((KTs[(t + 1) % 4], Vs[(t + 1) % 4], 1))
                keys.append((KTc[0], Vc[0], None))
                keys.append((KTc[1], Vc[1], None))
                nk = len(keys)

                def qk(kvh):
                    pts = [PTs[(kvh % 2) * 5 + i] for i in range(nk)]
                    for i, (KT, V, mi) in enumerate(keys):
                        bank = nps()
                        S.group(pe, [lambda e, KT=KT, bank=bank: e.matmul(
                            bank[:], KT[:, kvh, :], QT[:, kvh * 4:kvh * 4 + 4, :], start=True, stop=True)],
                            [KT, QT], [bank])
                        S.op(act, lambda e, bank=bank, i=i: e.activation(out=pts[i][:], in_=bank[:], func=AF.Exp,
                                                                         scale=0.125), [bank], [pts[i]])
                        if mi is not None:
                            S.op(dve, lambda e, i=i, mi=mi: e.tensor_tensor(out=pts[i][:], in0=pts[i][:],
                                                                           in1=mask[:, mi, :], op=ALU.mult),
                                 [pts[i], mask], [pts[i]])
                        if i % 2 == 1:
                            yield
                    yield

                def pv(kvh):
                    pts = [PTs[(kvh % 2) * 5 + i] for i in range(nk)]
                    obank = nps()
                    ov = obank[:, 0:260].rearrange("p (r d) -> p r d", r=4)
                    fns = []
                    for r in range(4):
                        for i, (KT, V, mi) in enumerate(keys):
                            fns.append(lambda e, r=r, i=i, V=V: e.matmul(
                                ov[:, r, :], pts[i][:, r * 128:(r + 1) * 128], V[:, kvh, :],
                                start=(i == 0), stop=(i == nk - 1)))
                    S.group(pe, fns, pts + [k_[1] for k_ in keys], [obank])
                    S.op(dve, lambda e, ov=ov: e.tensor_tensor(out=den[:, 0:4], in0=ov[:, :, 64],
                                                               in1=esink[:, kvh * 4:kvh * 4 + 4], op=ALU.add),
                         [obank, esink], [den])
                    S.op(dve, lambda e: e.reciprocal(out=den[:, 4:8], in_=den[:, 0:4]), [den], [den])
                    S.op(dve, lambda e, ov=ov: e.tensor_tensor(
                        out=yb[:, kvh * 256:(kvh + 1) * 256].rearrange("p (r d) -> p r d", r=4), in0=ov[:, :, 0:64],
                        in1=den[:, 4:8].unsqueeze(2).to_broadcast([128, 4, 64]), op=ALU.mult), [obank, den], [yb])
                    yield

                yield from qk(0)
                for kvh in range(4):
                    if kvh + 1 < 4:
                        yield from qk(kvh + 1)
                    yield from pv(kvh)

                def evac(bank, view, k0, n):
                    S.op(act, lambda e: e.activation(out=ybT[:, k0:k0 + n, :].rearrange("p a b -> p (a b)"),
                                                     in_=view, func=AF.Copy), [bank], [ybT])
                yield from transpose_evac(yb, 0, D, BF16, evac)

            def p2_B2(t):
                hT = hTs[t % 4]; ybT = ybTs[t % 2]; st = smBs[t % 2]; h2f = h2fs[t % 2]; mg = h2f
                x1 = xres
                load(sp, xres, xres[:], x_d[t * 128:(t + 1) * 128, :])
                load(sp, mat, mat[:], ma_d[t * 128:(t + 1) * 128, :], reads=[ma_b[t]])
                for h in range(2):
                    bank = nps()
                    sl = slice(h * 512, (h + 1) * 512)
                    mm_acc(bank, bank[:], [(hT[:, k, :], w2[:, k, 1536 + h * 512:1536 + (h + 1) * 512]) for k in range(8)]
                           + [(ones_b[0:1, :], bgb_row[0:1, sl])], [hT, w2, ones_b, bgb_row])
                    S.op(act, lambda e, bank=bank, sl=sl: e.activation(out=gb[:, sl], in_=bank[:], func=AF.Tanh,
                                                                       scale=0.5), [bank], [gb])
                    yield
                for h in range(2):
                    bank = nps()
                    sl = slice(h * 512, (h + 1) * 512)
                    mm_acc(bank, bank[:], [(ybT[:, k, :], wpb[:, k, sl]) for k in range(8)], [ybT, wpb])
                    S.op(dve, lambda e, bank=bank, sl=sl: e.scalar_tensor_tensor(
                        out=mg[:, sl], in0=gb[:, sl], scalar=1.0, in1=bank[:], op0=ALU.add, op1=ALU.mult),
                        [bank, gb], [mg])
                    S.op(dve, lambda e, sl=sl: e.tensor_tensor(out=mgb[:, sl], in0=mg[:, sl], in1=mat[:, sl],
                                                               op=ALU.add), [mg, mat], [mgb])
                    yield

                def evac2(bank, view, k0, n):
                    S.op(act, lambda e: e.activation(out=mgT[:, k0:k0 + n, :].rearrange("p a b -> p (a b)"),
                                                     in_=view, func=AF.Copy), [bank], [mgT])
                yield from transpose_evac(mgb, 0, D, BF16, evac2)
                for h in range(2):
                    bank = nps()
                    sl = slice(h * 512, (h + 1) * 512)
                    mm_acc(bank, bank[:], [(mgT[:, k, :], wo[:, k, sl]) for k in range(8)], [mgT, wo])
                    S.op(dve, lambda e, bank=bank, sl=sl: e.tensor_tensor(out=x1[:, sl], in0=bank[:], in1=x1[:, sl],
                                                                         op=ALU.add), [bank, x1], [x1])
                    yield
                S.dma(sp, lambda e: e.dma_start(out=acc_d[t * 128:(t + 1) * 128, :], in_=x1[:]),
                      [x1], [acc_b[t]], sembuf=x1)
                sumsq(x1, h2f, st, 2)
                rstd_from_ss(st, 2, 3)
                S.op(dve, lambda e: e.scalar_tensor_tensor(out=h2f[:], in0=x1[:], scalar=st[:, 3:4], in1=A2_bc[:],
                                                           op0=ALU.mult, op1=ALU.mult), [x1, st, A2_bc], [h2f])
                S.op(dve, lambda e: e.tensor_tensor(out=h2f[:], in0=h2f[:], in1=B2_bc[:], op=ALU.add),
                     [h2f, B2_bc], [h2f])
                S.op(dve, lambda e: e.tensor_copy(out=h2b[:], in_=h2f[:]), [h2f], [h2b])
                S.dma(sp, lambda e: e.dma_start(out=h2_d[t * 128:(t + 1) * 128, :], in_=h2b[:]),
                      [h2b], [h2_b[t]], sembuf=h2b)
                yield

            def p2_B3(t):
                st = smBs[t % 2]; h2f = h2fs[t % 2]

                def evac3(bank, view, k0, n):
                    S.op(act, lambda e: e.activation(out=h2T[:, k0:k0 + n, :].rearrange("p a b -> p (a b)"),
                                                     in_=view, func=AF.Copy), [bank], [h2T])
                yield from transpose_evac(h2f, 0, D, F32, evac3)
                bank = nps()
                mm_acc(bank, bank[:, 0:NE], [(h2T[:, k, :], wr[:, k, :]) for k in range(8)], [h2T, wr])
                S.op(dve, lambda e: e.tensor_reduce(out=st[:, 4:5], in_=bank[:, 0:NE], axis=AX.X, op=ALU.max),
                     [bank], [st])
                S.op(dve, lambda e: e.tensor_scalar(out=st[:, 4:5], in0=st[:, 4:5], scalar1=-1.0, scalar2=None,
                                                    op0=ALU.mult), [st], [st])
                S.op(dve, lambda e: e.memset(st[:, 5:6], 0.0), [], [st])
                S.op(act, lambda e: e.activation(out=lg[:], in_=bank[:, 0:NE], func=AF.Exp, bias=st[:, 4:5],
                                                 accum_out=st[:, 5:6]), [bank, st], [lg, st])
                S.op(dve, lambda e: e.reciprocal(out=st[:, 6:7], in_=st[:, 5:6]), [st], [st])
                S.op(dve, lambda e: e.tensor_scalar(out=aff_all[:, t, :], in0=lg[:], scalar1=st[:, 6:7], scalar2=None,
                                                    op0=ALU.mult), [lg, st], [aff_all])
                S.op(dve, lambda e: e.tensor_copy(out=lg2[:], in_=aff_all[:, t, :]), [aff_all], [lg2])
                S.dma(sp, lambda e: e.dma_start(out=aff_d[t * 128:(t + 1) * 128, :], in_=lg2[:]),
                      [lg2], [affd_b[t]], sembuf=lg2)

            print("P2 sbuf left", nc.sbuf_bytes_remaining)
            p2_pro(0)
            for i in range(NT + 4):
                cast_some(int(len(cast_jobs) * (34 + i) / 66.0))
                interleave([p2_A(i) if i < NT else None,
                            p2_B1(i - 2) if 0 <= i - 2 < NT else None,
                            p2_B2(i - 3) if 0 <= i - 3 < NT else None,
                            p2_B3(i - 4) if 0 <= i - 4 < NT else None],
                           pools=[(0, 1), (2, 3, 4, 5), (6, 7), (6, 7)])
            cast_some(len(cast_jobs))
            S.end_phase()

        with ExitStack() as ph:
            lo = sb(ph, "lo", [128, NE]); hi = sb(ph, "hi", [128, NE]); mid = sb(ph, "mid", [128, NE])
            cmp_ = sb(ph, "cmp", [128, NT, NE]); cnt = sb(ph, "cnt", [128, NE])
            pred = sb(ph, "pred", [128, NE]); npred = sb(ph, "npred", [128, NE]); tq = sb(ph, "tq", [128, NE])
            selb = sb(ph, "selb", [128, NT, NE], BF16)
            csf = sb(ph, "csf", [128, NT, NE]); csb = sb(ph, "csb", [128, NT, NE], BF16)
            tri_f = sb(ph, "tri_f", [128, 128]); tri_b = sb(ph, "tri_b", [128, 128], BF16)
            iota = sb(ph, "iota", [128, 128]); pidx = sb(ph, "pidx", [128, 1])
            pos = sb(ph, "pos", [128, NT, NE]); posi = sb(ph, "posi", [128, NT, NE], I32)
            clo_i = sb(ph, "clo_i", [128, NT, NE], I32); chi_i = sb(ph, "chi_i", [128, NT, NE], I32)
            clo = sb(ph, "clo", [128, NT, NE]); chi = sb(ph, "chi", [128, NT, NE])
            As = [sb(ph, "A%d" % i, [128, NE, 128], BF16) for i in range(3)]
            Bs = [sb(ph, "B%d" % i, [128, NE, 4], BF16) for i in range(2)]
            Rs = [sb(ph, "R%d" % i, [128, NE, 8], BF16) for i in range(2)]
            idxf = sb(ph, "idxf", [128, NE, 4])
            load(sp, tri_f, tri_f[:], tri_d)
            load(sp, iota, iota[:], iota_d)
            load(sp, pidx, pidx[:], pidx_d)
            S.op(dve, lambda e: e.tensor_copy(out=tri_b[:], in_=tri_f[:]), [tri_f], [tri_b])
            S.op(dve, lambda e: e.memset(lo[:], 0.0), [], [lo])
            affv = aff_all[:]
            for it in range(NBIS):
                wk = 2.0 ** (-(it + 1))
                S.op(dve, lambda e, wk=wk: e.tensor_scalar(out=mid[:], in0=lo[:], scalar1=wk, scalar2=None,
                                                           op0=ALU.add), [lo], [mid])
                S.op(dve, lambda e: e.tensor_tensor(out=cmp_[:], in0=affv,
                                                    in1=mid[:].unsqueeze(1).to_broadcast([128, NT, NE]), op=ALU.is_ge),
                     [aff_all, mid], [cmp_])
                S.op(dve, lambda e: e.tensor_reduce(out=cnt[:], in_=cmp_[:].rearrange("p t e -> p e t"), axis=AX.X,
                                                    op=ALU.add), [cmp_], [cnt])
                bank = nps()
                mm_acc(bank, bank[:, 0:NE], [(ones_f[:], cnt[:])], [ones_f, cnt])
                S.op(dve, lambda e, bank=bank: e.tensor_scalar(out=pred[:], in0=bank[:, 0:NE], scalar1=float(CAP) - 0.5,
                                                               scalar2=None, op0=ALU.is_ge), [bank], [pred])
                S.op(dve, lambda e: e.tensor_tensor(out=tq[:], in0=mid[:], in1=pred[:], op=ALU.mult), [mid, pred], [tq])
                S.op(dve, lambda e: e.tensor_tensor(out=lo[:], in0=lo[:], in1=tq[:], op=ALU.max), [lo, tq], [lo])
            S.op(dve, lambda e: e.tensor_tensor(out=cmp_[:], in0=affv, in1=lo[:].unsqueeze(1).to_broadcast([128, NT, NE]),
                                                op=ALU.is_ge), [aff_all, lo], [cmp_])
            S.op(dve, lambda e: e.tensor_copy(out=selb[:], in_=cmp_[:]), [cmp_], [selb])
            S.op(dve, lambda e: e.memset(csf[:, 0, :], 0.0), [], [csf])
            for t in range(1, NT):
                S.op(dve, lambda e, t=t: e.tensor_tensor(out=csf[:, t, :], in0=csf[:, t - 1, :], in1=cmp_[:, t - 1, :],
                                                         op=ALU.add), [csf, cmp_], [csf])
            S.op(dve, lambda e: e.tensor_copy(out=csb[:], in_=csf[:]), [csf], [csb])
            bank = nps()
            fl = "p t e -> p (t e)"
            S.group(pe, [lambda e: e.matmul(bank[:], tri_b[:], selb[:].rearrange(fl), start=True, stop=False),
                         lambda e: e.matmul(bank[:], ones_b[:], csb[:].rearrange(fl), start=False, stop=True)],
                    [tri_b, selb, ones_b, csb], [bank])
            S.op(dve, lambda e: e.tensor_scalar(out=cmp_[:], in0=cmp_[:], scalar1=-8192.0, scalar2=8192.0, op0=ALU.mult,
                                                op1=ALU.add), [cmp_], [cmp_])
            S.op(dve, lambda e: e.tensor_tensor(out=pos[:].rearrange(fl), in0=bank[:], in1=cmp_[:].rearrange(fl),
                                                op=ALU.add), [bank, cmp_], [pos])
            S.op(dve, lambda e: e.tensor_copy(out=posi[:], in_=pos[:]), [pos], [posi])
            S.op(dve, lambda e: e.tensor_single_scalar(out=clo_i[:], in_=posi[:], scalar=7, op=ALU.arith_shift_right),
                 [posi], [clo_i])
            S.op(dve, lambda e: e.tensor_single_scalar(out=chi_i[:], in_=posi[:], scalar=127, op=ALU.bitwise_and),
                 [posi], [chi_i])
            S.op(dve, lambda e: e.tensor_copy(out=clo[:], in_=clo_i[:]), [clo_i], [clo])
            S.op(dve, lambda e: e.tensor_copy(out=chi[:], in_=chi_i[:]), [chi_i], [chi])
            idxs = sb(ph, "idxs", [128, NE, 8])
            S.op(dve, lambda e: e.memset(idxs[:], 0.0), [], [idxs])
            for t in range(NT):
                A = As[t % 3]; B = Bs[t % 2]; R = Rs[t % 2]
                S.op(dve, lambda e, t=t, A=A: e.tensor_tensor(
                    out=A[:], in0=chi[:, t, :].unsqueeze(2).to_broadcast([128, NE, 128]),
                    in1=iota[:].unsqueeze(1).to_broadcast([128, NE, 128]), op=ALU.is_equal), [chi, iota], [A])
                S.op(dve, lambda e, t=t, B=B: e.tensor_tensor(
                    out=B[:], in0=clo[:, t, :].unsqueeze(2).to_broadcast([128, NE, 4]),
                    in1=iota[:, 0:4].unsqueeze(1).to_broadcast([128, NE, 4]), op=ALU.is_equal), [clo, iota], [B])
                S.op(dve, lambda e, t=t, B=B, R=R: e.tensor_scalar(out=R[:, :, 0:4], in0=B[:], scalar1=float(t),
                                                                   scalar2=None, op0=ALU.mult), [B], [R])
                S.op(dve, lambda e, B=B, R=R: e.tensor_scalar(out=R[:, :, 4:8], in0=B[:], scalar1=pidx[:, 0:1],
                                                              scalar2=None, op0=ALU.mult), [B, pidx], [R])
                ibank = nps()
                iv = ibank[:, 0:NE * 8].rearrange("p (e c) -> p e c", e=NE)
                fns = [lambda e, ex=ex, A=A, R=R, iv=iv: e.matmul(iv[:, ex, :], A[:, ex, :], R[:, ex, :],
                                                                  start=True, stop=True)
                       for ex in range(NE)]
                S.group(pe, fns, [A, R], [ibank])
                S.op(dve, lambda e, iv=iv: e.tensor_tensor(out=idxs[:], in0=iv, in1=idxs[:], op=ALU.add),
                     [ibank, idxs], [idxs])
            S.op(dve, lambda e: e.scalar_tensor_tensor(out=idxf[:], in0=idxs[:, :, 0:4], scalar=128.0, in1=idxs[:, :, 4:8],
                                                       op0=ALU.mult, op1=ALU.add), [idxs], [idxf])
            S.op(dve, lambda e: e.tensor_copy(out=idx_i[:], in_=idxf[:]), [idxf], [idx_i])
            if debug:
                S.dma(sp, lambda e: e.dma_start(out=dbg_d[:, 32:48], in_=lo[:]), [lo], [dbg_b], sembuf=lo)
                S.dma(sp, lambda e: e.dma_start(out=dbg_d[:, 64:128], in_=idxf[:].rearrange("p a b -> p (a b)")),
                      [idxf], [dbg_b], sembuf=idxf)
            S.end_phase()

        with ExitStack() as ph:
            wgu = [sb(ph, "wgu%d" % i, [128, 2, 8, 512], BF16) for i in range(4)]
            wd = [sb(ph, "wd%d" % i, [128, 16, D], BF16) for i in range(2)]
            xe = [[sb(ph, "xe%d_%d" % (i, c), [128, D], BF16) for c in range(4)] for i in range(2)]
            affg = [[sb(ph, "affg%d_%d" % (i, c), [128, NE]) for c in range(4)] for i in range(3)]
            xeT = sb(ph, "xeT", [128, 8, 512], BF16)
            sg = [sb(ph, "sg%d" % i, [128, 512]) for i in range(2)]
            hTm = sb(ph, "hTm", [128, 16, 512], BF16)
            ysb = [sb(ph, "ysb%d" % i, [128, D]) for i in range(4)]
            def load_unit(gu):
                ex, u = gu // 4, gu % 4
                w = wgu[gu % 4]
                S.dma(sp, lambda e: e.dma_start(out=w[:, 0, :, :], in_=wg_s[ex, u]), [], [w], sembuf=w)
                S.dma(sp, lambda e: e.dma_start(out=w[:, 1, :, :], in_=wu_s[ex, u]), [], [w], sembuf=w)

            def load_wd(ex):
                w = wd[ex % 2]
                for hh in range(2):
                    S.dma(sp, lambda e, hh=hh: e.dma_start(out=w[:, hh * 8:(hh + 1) * 8, :],
                                                           in_=wd_s[ex][:, hh * 8:(hh + 1) * 8, :]),
                          [], [w], sembuf=w)

            def gather(ex):
                for c in range(4):
                    xt_ = xe[ex % 2][c]
                    S.dma(pool, lambda e, c=c, xt_=xt_: e.indirect_dma_start(
                        out=xt_[:], out_offset=None, in_=h2_d,
                        in_offset=bass.IndirectOffsetOnAxis(ap=idx_i[:, ex, c:c + 1], axis=0)),
                        [idx_i] + h2_b, [xt_], sembuf=xt_)
                    ag = affg[ex % 3][c]
                    S.dma(pool, lambda e, c=c, ag=ag: e.indirect_dma_start(
                        out=ag[:], out_offset=None, in_=aff_d,
                        in_offset=bass.IndirectOffsetOnAxis(ap=idx_i[:, ex, c:c + 1], axis=0)),
                        [idx_i] + affd_b, [ag], sembuf=ag)

            print("P4 sbuf left", nc.sbuf_bytes_remaining)
            gather(0)
            load_unit(0)
            load_unit(1)
            load_wd(0)
            load_unit(2)
            gather(1)
            yi = 0
            pending = []

            scat_b = [S.buf("scat%d" % i) for i in range(NE)]

            def flush_scatters():
                for (ex_, c_, y__) in pending:
                    rd = [y__, idx_i] + ([scat_b[ex_ - 1]] if ex_ > 0 else [])
                    S.dma(pool, lambda e, ex_=ex_, c_=c_, y__=y__: e.indirect_dma_start(
                        out=acc_d, out_offset=bass.IndirectOffsetOnAxis(ap=idx_i[:, ex_, c_:c_ + 1], axis=0),
                        in_=y__[:], in_offset=None, compute_op=ALU.add),
                        rd, [], sembuf=y__)
                    s_ = y__.sem
                    scat_b[ex_].w[id(s_)] = (s_, S.semval[s_])
                del pending[:]

            for ex in range(NE):
                for k in range(8):
                    bank = nps()
                    vw = bank[:].bitcast(BF16)
                    fns = [lambda e, c=c, vw=vw, k=k: e.transpose(vw[:, c * 128:(c + 1) * 128],
                                                                  xe[ex % 2][c][:, k * 128:(k + 1) * 128], ident_b[:])
                           for c in range(4)]
                    S.group(pe, fns, xe[ex % 2] + [ident_b], [bank])
                    if k % 2 == 0:
                        S.op(act, lambda e, vw=vw, k=k: e.activation(out=xeT[:, k, :], in_=vw[:, 0:512], func=AF.Copy),
                             [bank], [xeT])
                    else:
                        S.op(dve, lambda e, vw=vw, k=k: e.tensor_copy(out=xeT[:, k, :], in_=vw[:, 0:512]), [bank], [xeT])
                for u in range(4):
                    gu = ex * 4 + u
                    if gu + 3 < NE * 4:
                        load_unit(gu + 3)
                    if u == 1:
                        flush_scatters()
                    if u == 2 and ex + 2 < NE:
                        gather(ex + 2)
                    if u == 3 and ex + 1 < NE:
                        load_wd(ex + 1)
                    w = wgu[gu % 4]
                    for f in range(4):
                        fc = u * 4 + f
                        gb_ = nps()
                        mm_acc(gb_, gb_[:], [(w[:, 0, k, f * 128:(f + 1) * 128], xeT[:, k, :]) for k in range(8)], [w, xeT])
                        ub_ = nps()
                        mm_acc(ub_, ub_[:], [(w[:, 1, k, f * 128:(f + 1) * 128], xeT[:, k, :]) for k in range(8)], [w, xeT])
                        s_ = sg[fc % 2]
                        S.op(act, lambda e, gb_=gb_, s_=s_: e.activation(out=s_[:], in_=gb_[:], func=AF.Silu), [gb_], [s_])
                        S.op(dve, lambda e, ub_=ub_, s_=s_, fc=fc: e.tensor_tensor(out=hTm[:, fc, :], in0=ub_[:], in1=s_[:],
                                                                               op=ALU.mult), [ub_, s_], [hTm])
                wdt = wd[ex % 2]
                for c in range(4):
                    y_ = ysb[yi % 4]
                    yi += 1
                    ag = affg[ex % 3][c]
                    for h in range(2):
                        bank = nps()
                        sl = slice(h * 512, (h + 1) * 512)
                        mm_acc(bank, bank[:], [(hTm[:, fc, c * 128:(c + 1) * 128], wdt[:, fc, sl]) for fc in range(16)],
                               [hTm, wdt])
                        S.op(act, lambda e, bank=bank, sl=sl, y_=y_, ag=ag: e.activation(
                            out=y_[:, sl], in_=bank[:], func=AF.Copy, scale=ag[:, ex:ex + 1]), [bank, ag], [y_])
                        S.op(dve, lambda e, sl=sl, y_=y_: e.tensor_tensor(out=y_[:, sl], in0=y_[:, sl], in1=g2_bc[:, sl],
                                                                         op=ALU.mult), [y_, g2_bc], [y_])
                    pending.append((ex, c, y_))
            flush_scatters()
            S.end_phase()

        with ExitStack() as ph:
            G = 4
            fg = sb(ph, "fg", [128, D])
            xin = [sb(ph, "xin%d" % i, [128, D]) for i in range(2 * G)]
            xo = [sb(ph, "xo%d" % i, [128, D]) for i in range(2 * G)]
            junk = sb(ph, "junk", [128, D])
            sm = [sb(ph, "sm%d" % i, [128, 32]) for i in range(2)]
            load(sp, fg, fg[:], fg_d)
            for g in range(NT // G):
                st = sm[g % 2]
                S.op(dve, lambda e, st=st: e.memset(st[:, 0:G], 0.0), [], [st])
                for j in range(G):
                    t = g * G + j
                    xi = xin[t % (2 * G)]
                    load(sp, xi, xi[:], acc_d[t * 128:(t + 1) * 128, :], reads=[acc_b[t]])
                    S.op(act, lambda e, xi=xi, st=st, j=j: e.activation(out=junk[:], in_=xi[:], func=AF.Square,
                                                                        accum_out=st[:, j:j + 1]), [xi], [junk, st])
                rstd_from_ss(st, 0, G, n=G, sc=2 * G)
                for j in range(G):
                    t = g * G + j
                    xi = xin[t % (2 * G)]; xo_ = xo[t % (2 * G)]
                    S.op(dve, lambda e, xi=xi, xo_=xo_, st=st, j=j: e.scalar_tensor_tensor(
                        out=xo_[:], in0=xi[:], scalar=st[:, G + j:G + j + 1], in1=fg[:], op0=ALU.mult, op1=ALU.mult),
                        [xi, st, fg], [xo_])
                    S.dma(sp, lambda e, xo_=xo_, t=t: e.dma_start(out=y_d[t * 128:(t + 1) * 128, :], in_=xo_[:]),
                          [xo_], [], sembuf=xo_)
            S.end_phase()
    return nc


_NC_CACHE = {}


def _consts():
    c = {}
    tok = np.arange(128)[:, None] + 128 * np.arange(NT)[None, :]
    row = (tok // 64).astype(np.float64)
    col = (tok % 64).astype(np.float64)
    fr = 10000.0 ** (-np.arange(0, 32, 2, dtype=np.float64) / 32)
    ang = np.stack([row[..., None] * fr, col[..., None] * fr], axis=2)
    c["rope_cos"] = np.cos(ang).astype(np.float32)
    c["rope_sin"] = np.sin(ang).astype(np.float32)
    j = np.arange(128)[:, None]
    i = np.arange(128)[None, :]
    m0 = (j >= i).astype(np.float32)
    m1 = (j <= i).astype(np.float32)
    c["masks"] = np.stack([np.tile(m0, (1, 4)), np.tile(m1, (1, 4))], axis=1).astype(np.float32)
    c["ident"] = np.eye(128, dtype=np.float32)
    c["iota"] = np.tile(np.arange(128, dtype=np.float32)[None, :], (128, 1))
    c["pidx"] = np.arange(128, dtype=np.float32)[:, None].copy()
    c["tri"] = (j < i).astype(np.float32)
    return c


def _prep_inputs(inp):
    f = lambda a: np.ascontiguousarray(np.asarray(a, dtype=np.float32))
    bc = lambda v: np.ascontiguousarray(np.broadcast_to(np.asarray(v, np.float32)[None, :], (128, v.shape[-1])))
    fp = lambda v: np.ascontiguousarray(np.asarray(v, np.float32).reshape(-1, 128).T)
    shared = {}
    b_ada = np.asarray(inp["b_ada"], np.float32)[0]
    shared["w_ada"] = f(inp["w_ada"][0])
    shared["bada_fp"] = fp(b_ada[:2 * D])
    shared["bada_bc"] = bc(b_ada[2 * D:])
    shared["n1g_fp"] = fp(np.asarray(inp["norm1_g"])[0])
    shared["n2g_bc"] = bc(np.asarray(inp["norm2_g"])[0])
    shared["fg_bc"] = bc(np.asarray(inp["final_g"]))
    shared["lng_bc"] = bc(np.asarray(inp["gmlp_ln_g"])[0])
    shared["lnb_bc"] = bc(np.asarray(inp["gmlp_ln_b"])[0])
    shared["bmg_bc"] = bc(np.asarray(inp["b_merge_gate"])[0])
    ws = np.asarray(inp["w_spatial"], np.float32)[0]
    shared["wsT"] = np.ascontiguousarray(ws.transpose(2, 0, 1))
    shared["wsP"] = np.ascontiguousarray(ws.transpose(1, 0, 2))
    shared["bs_pg"] = np.ascontiguousarray(np.asarray(inp["b_spatial"], np.float32)[0].T)
    shared["sink_bc"] = bc(np.asarray(inp["attn_sink"])[0])
    wr = np.asarray(inp["w_router"], np.float32)[0]
    shared["wr"] = np.ascontiguousarray(wr.reshape(8, 128, NE).transpose(1, 0, 2))
    shared["w_in"] = f(inp["w_in"][0])
    shared["w_proj_a"] = f(inp["w_proj_a"][0])
    shared["w_proj_b"] = f(inp["w_proj_b"][0])
    shared["w_out"] = f(inp["w_out"][0])
    shared["w_exp_gate"] = f(inp["w_exp_gate"][0])
    shared["w_exp_up"] = f(inp["w_exp_up"][0])
    shared["w_exp_down"] = f(inp["w_exp_down"][0])
    shared.update(_consts())
    x = np.asarray(inp["x"], np.float32)
    c = np.asarray(inp["c"], np.float32)
    ctx = np.asarray(inp["ctx"], np.float32)
    c_ctx = np.asarray(inp["c_ctx"], np.float32)
    maps = []
    for b in range(x.shape[0]):
        m = dict(shared)
        m["x"] = np.ascontiguousarray(x[b])
        m["ctx"] = np.ascontiguousarray(ctx[b])
        cc = np.stack([c[b].reshape(8, 128).T, c_ctx.reshape(8, 128).T], axis=2)
        m["cc"] = np.ascontiguousarray(cc)
        maps.append(m)
    return maps


def kernel(**inputs):
    maps = _prep_inputs(inputs)
    if "nc" not in _NC_CACHE:
        _NC_CACHE["nc"] = _build(False)
    nc = _NC_CACHE["nc"]
    res = run_bass_kernel_spmd(nc, maps, core_ids=list(range(len(maps))))
    return np.stack([np.asarray(r["y"], dtype=np.float32) for r in res.results], axis=0)
```
